# Optimizing a Trainium2 kernel written in Bass

```python
import math
import jax, jax.numpy as jnp
from jax import lax
import numpy as np

D_MODEL = 1024
BATCH = 8
SEQ = 2048
DEPTH = 1

D_MIX = D_MODEL
HEAD_DIM = 64
SB_HEADS = 8
SB_WIDTH = SB_HEADS * HEAD_DIM
SG_HEADS = 8
SG_WIDTH = SG_HEADS * HEAD_DIM
D_IN = 3 * SB_WIDTH + 2 * SG_WIDTH
CHUNK = 128
Q_BLOCK = 128
N_GROUPS = 4
EXPERTS_PER_GROUP = 8
TOP_K = 2
D_EXPERT = 512
EPS = 1e-6

kernel_name = "hymba_style_stickbreak_sgmlp_hiermoe"


def rmsnorm(x, g):
    xf = x.astype(jnp.float32)
    y = xf * lax.rsqrt(jnp.mean(xf * xf, axis=-1, keepdims=True) + EPS)
    return (y * g.astype(jnp.float32)).astype(x.dtype)


def stick_breaking_attention(q, k, v):
    B, S, H, Dh = q.shape
    nb = S // Q_BLOCK
    scale = 1.0 / math.sqrt(Dh)
    k = k.transpose(0, 2, 1, 3)
    v = v.transpose(0, 2, 1, 3)
    q_blocks = q.transpose(0, 2, 1, 3).reshape(B, H, nb, Q_BLOCK, Dh).transpose(2, 0, 1, 3, 4)
    key_pos = jnp.arange(S)

    def one_block(args):
        qb, blk = args
        z = jnp.einsum('bhqd,bhkd->bhqk', qb, k).astype(jnp.float32) * scale
        q_pos = blk * Q_BLOCK + jnp.arange(Q_BLOCK)
        mask = key_pos[None, :] < q_pos[:, None]
        log_beta = jax.nn.log_sigmoid(z)
        log_1m_beta = jnp.where(mask, jax.nn.log_sigmoid(-z), 0.0)
        after = lax.cumsum(log_1m_beta, axis=log_1m_beta.ndim - 1, reverse=True) - log_1m_beta
        a = jnp.where(mask, jnp.exp(log_beta + after), 0.0)
        return jnp.einsum('bhqk,bhkd->bhqd', a.astype(v.dtype), v)

    out = lax.map(one_block, (q_blocks, jnp.arange(nb)))
    return out.transpose(1, 0, 3, 2, 4).reshape(B, S, H * Dh)


def chunked_spatial_gating(u, vg, sg_norm_g, w_spatial, b_spatial):
    B, S, _ = u.shape
    nc = S // CHUNK
    u = jax.nn.gelu(u)
    vg = rmsnorm(jax.nn.gelu(vg), sg_norm_g)
    vg = vg.reshape(B, nc, CHUNK, SG_HEADS, HEAD_DIM)
    causal = jnp.tril(jnp.ones((CHUNK, CHUNK), dtype=w_spatial.dtype))
    w_masked = w_spatial * causal[None]
    mixed = jnp.einsum('hts,bcshd->bcthd', w_masked, vg) + b_spatial.T[None, None, :, :, None]
    return u * mixed.reshape(B, S, SG_WIDTH)


def hierarchical_moe(x, w_router_group, b_router_group, w_router_expert, b_router_expert,
                     w_gate, w_up, w_down):
    B, S, D = x.shape
    xt = x.reshape(-1, D)
    n = xt.shape[0]
    g_logits = (xt @ w_router_group).astype(jnp.float32) + b_router_group.astype(jnp.float32)
    g_probs = jax.nn.softmax(g_logits, axis=-1)
    g_idx = jnp.argmax(g_logits, axis=-1)
    g_onehot = jax.nn.one_hot(g_idx, N_GROUPS, dtype=jnp.float32)
    g_weight = jnp.sum(g_probs * g_onehot, axis=-1)
    e_logits_all = jnp.einsum('nd,gde->nge', xt, w_router_expert).astype(jnp.float32) \
        + b_router_expert.astype(jnp.float32)[None]
    e_logits = jnp.sum(e_logits_all * g_onehot[:, :, None], axis=1)
    e_probs = jax.nn.softmax(e_logits, axis=-1)
    top_p, top_i = lax.top_k(e_probs, TOP_K)
    top_p = top_p / jnp.sum(top_p, axis=-1, keepdims=True)
    e_w = jnp.sum(jax.nn.one_hot(top_i, EXPERTS_PER_GROUP, dtype=jnp.float32) * top_p[..., None], axis=1)
    combine = (g_onehot[:, :, None] * (g_weight[:, None] * e_w)[:, None, :]).astype(x.dtype)
    out = jnp.zeros((n, D), dtype=x.dtype)
    for g in range(N_GROUPS):
        h = jax.nn.silu(jnp.einsum('nd,edf->nef', xt, w_gate[g])) * jnp.einsum('nd,edf->nef', xt, w_up[g])
        out = out + jnp.einsum('nef,efd->nd', h * combine[:, g, :, None], w_down[g])
    return out.reshape(B, S, D)


def setup_inputs(seed: int = 0) -> dict:
    key = jax.random.key(seed)
    ks = jax.random.split(key, 20)
    f32 = jnp.float32
    nrm = lambda k, shape, s: jax.random.normal(k, shape, f32) * s
    gain = lambda k, shape: 1.0 + 0.02 * jax.random.normal(k, shape, f32)
    L, G, E = DEPTH, N_GROUPS, EXPERTS_PER_GROUP
    return {
        "x": jax.random.normal(ks[0], (BATCH, SEQ, D_MODEL), f32),
        "attn_norm_g": gain(ks[1], (L, D_MODEL)),
        "w_in": nrm(ks[2], (L, D_MODEL, D_IN), D_MODEL ** -0.5),
        "sg_norm_g": gain(ks[3], (L, SG_WIDTH)),
        "w_spatial": nrm(ks[4], (L, SG_HEADS, CHUNK, CHUNK), CHUNK ** -0.5),
        "b_spatial": gain(ks[5], (L, SG_HEADS, CHUNK)),
        "sb_out_norm_g": gain(ks[6], (L, SB_WIDTH)),
        "sg_out_norm_g": gain(ks[7], (L, SG_WIDTH)),
        "w_out": nrm(ks[8], (L, D_MIX, D_MODEL), D_MIX ** -0.5),
        "ffn_norm_g": gain(ks[9], (L, D_MODEL)),
        "w_router_group": nrm(ks[10], (L, D_MODEL, G), D_MODEL ** -0.5),
        "b_router_group": nrm(ks[11], (L, G), 0.01),
        "w_router_expert": nrm(ks[12], (L, G, D_MODEL, E), D_MODEL ** -0.5),
        "b_router_expert": nrm(ks[13], (L, G, E), 0.01),
        "w_gate": nrm(ks[14], (L, G, E, D_MODEL, D_EXPERT), D_MODEL ** -0.5),
        "w_up": nrm(ks[15], (L, G, E, D_MODEL, D_EXPERT), D_MODEL ** -0.5),
        "w_down": nrm(ks[16], (L, G, E, D_EXPERT, D_MODEL), D_EXPERT ** -0.5),
        "final_norm_g": gain(ks[17], (D_MODEL,)),
    }


def reference(x, attn_norm_g, w_in, sg_norm_g, w_spatial, b_spatial, sb_out_norm_g, sg_out_norm_g,
              w_out, ffn_norm_g, w_router_group, b_router_group, w_router_expert, b_router_expert,
              w_gate, w_up, w_down, final_norm_g):
    B, S, _ = x.shape
    h = x
    for layer in range(DEPTH):
        hn = rmsnorm(h, attn_norm_g[layer])
        proj = hn @ w_in[layer]
        q, k, v, u, vg = jnp.split(proj, [SB_WIDTH, 2 * SB_WIDTH, 3 * SB_WIDTH, 3 * SB_WIDTH + SG_WIDTH], axis=-1)
        shp = (B, S, SB_HEADS, HEAD_DIM)
        sb_out = stick_breaking_attention(q.reshape(shp), k.reshape(shp), v.reshape(shp))
        sg_out = chunked_spatial_gating(u, vg, sg_norm_g[layer], w_spatial[layer], b_spatial[layer])
        mixed = jnp.concatenate([rmsnorm(sb_out, sb_out_norm_g[layer]),
                                 rmsnorm(sg_out, sg_out_norm_g[layer])], axis=-1)
        h = h + mixed @ w_out[layer]
        hn = rmsnorm(h, ffn_norm_g[layer])
        h = h + hierarchical_moe(hn, w_router_group[layer], b_router_group[layer],
                                 w_router_expert[layer], b_router_expert[layer],
                                 w_gate[layer], w_up[layer], w_down[layer])
    return rmsnorm(h, final_norm_g)
```

```python
import numpy as np
from contextlib import ExitStack
import concourse.bass as bass
import concourse.mybir as mybir
from concourse.bass_utils import run_bass_kernel_spmd

F32 = mybir.dt.float32
BF16 = mybir.dt.bfloat16
I32 = mybir.dt.int32
AF = mybir.ActivationFunctionType
ALU = mybir.AluOpType
AX = mybir.AxisListType

P = 128
T = 2048
NT = 16
D = 1024
KC = 8
EPS = 1e-6


class Buf:
    def __init__(self, name, after=()):
        self.name = name
        self.w = None
        self.r = {}
        self.dsem = None
        self.dcnt = 0
        for b in after:
            if b.w is not None:
                self.add_read(b.w)
            for t in b.r.values():
                self.add_read(t)

    def add_read(self, tok):
        key = id(tok[0])
        old = self.r.get(key)
        if old is None or old[1] < tok[1]:
            self.r[key] = tok


class Sched:
    COMPUTE = ("pe", "act", "dve", "pool")

    def __init__(self, nc, es, same_engine_sync=True):
        self.nc = nc
        self.es = es
        self.E = {"pe": nc.tensor, "act": nc.scalar, "dve": nc.vector, "pool": nc.gpsimd, "sp": nc.sync}
        self.csem = {k: es.enter_context(nc.semaphore("cs_" + k)) for k in self.COMPUTE}
        self.ccnt = {k: 0 for k in self.COMPUTE}
        self.waited = {k: {} for k in self.E}
        self.same = same_engine_sync
        self.nsem = 4
        self.all_dma_toks = []

    def _wait(self, eng, tok):
        sem, val, prod = tok
        if prod == eng and (eng == "pe" or not self.same):
            return
        key = id(sem)
        if self.waited[eng].get(key, 0) >= val:
            return
        if prod == eng and prod in self.COMPUTE:
            pass
        self.E[eng].wait_ge(sem, val)
        self.waited[eng][key] = val

    def _deps(self, eng, reads, writes):
        for b in reads:
            if b.w is not None:
                self._wait(eng, b.w)
        for b in writes:
            if b.w is not None:
                self._wait(eng, b.w)
            for t in b.r.values():
                self._wait(eng, t)

    def op(self, eng, fn, reads=(), writes=()):
        self._deps(eng, reads, writes)
        inst = fn(self.E[eng])
        self.ccnt[eng] += 1
        inst.then_inc(self.csem[eng], 1)
        tok = (self.csem[eng], self.ccnt[eng], eng)
        for b in reads:
            b.add_read(tok)
        for b in writes:
            b.w = tok
            b.r = {}
        return tok

    def ops(self, eng, fns, reads=(), writes=()):
        self._deps(eng, reads, writes)
        inst = None
        for fn in fns:
            inst = fn(self.E[eng])
        self.ccnt[eng] += 1
        inst.then_inc(self.csem[eng], 1)
        tok = (self.csem[eng], self.ccnt[eng], eng)
        for b in reads:
            b.add_read(tok)
        for b in writes:
            b.w = tok
            b.r = {}
        return tok

    def _bufsem(self, b):
        if b.dsem is None:
            b.dsem = self.es.enter_context(self.nc.semaphore("ds_%d" % self.nsem))
            self.nsem += 1
        return b.dsem

    def dma(self, q, out, in_, sbuf_buf, reads=(), writes=(), **kw):
        self._deps(q, reads, writes)
        sem = self._bufsem(sbuf_buf)
        inst = self.E[q].dma_start(out=out, in_=in_, **kw)
        sbuf_buf.dcnt += 16
        inst.then_inc(sem, 16)
        tok = (sem, sbuf_buf.dcnt, "dma")
        for b in reads:
            b.add_read(tok)
        for b in writes:
            b.w = tok
            b.r = {}
        self.all_dma_toks.append(tok)
        return tok

    def idma(self, out, out_off, in_, in_off, sbuf_buf, reads=(), writes=(), bounds_check=None):
        self._deps("pool", reads, writes)
        sem = self._bufsem(sbuf_buf)
        if not hasattr(self, "_bregs"):
            self._bregs = {}
        if bounds_check not in self._bregs:
            self._bregs[bounds_check] = self.nc.gpsimd.to_reg(bounds_check)
        bounds_check = self._bregs[bounds_check]
        inst = self.nc.gpsimd.indirect_dma_start(
            out=out, out_offset=(bass.IndirectOffsetOnAxis(ap=out_off, axis=0) if out_off is not None else None),
            in_=in_, in_offset=(bass.IndirectOffsetOnAxis(ap=in_off, axis=0) if in_off is not None else None),
            bounds_check=bounds_check, oob_is_err=False)
        sbuf_buf.dcnt += 16
        inst.then_inc(sem, 16)
        tok = (sem, sbuf_buf.dcnt, "dma")
        for b in reads:
            b.add_read(tok)
        for b in writes:
            b.w = tok
            b.r = {}
        self.all_dma_toks.append(tok)
        return tok

    def wait_tok(self, eng, tok):
        self._wait(eng, tok)


def _rd_update(b, tok):
    b.r.append(tok)


def build_nc(stage="full", n_experts=32):
    DBG = stage.split(":")[1] if ":" in stage else "wxcy"
    stage = stage.split(":")[0]
    nc = bass.Bass("TRN2", target_bir_lowering=False)
    dt_in = lambda name, shape: nc.dram_tensor(name, shape, F32, kind="ExternalInput").ap()
    x = dt_in("x", [T, D])
    w_in = dt_in("w_in", [D, 2560])
    w_out = dt_in("w_out", [D, D])
    w_gate = dt_in("w_gate", [8192, 2048])
    w_up = dt_in("w_up", [8192, 2048])
    w_down = dt_in("w_down", [8192, 2048])
    w_sp = dt_in("w_spatial", [8, 128, 128])
    b_sp = dt_in("b_spatial", [8, 128])
    g_attn = dt_in("attn_norm_g", [D])
    g_ffn = dt_in("ffn_norm_g", [D])
    g_fin = dt_in("final_norm_g", [D])
    g_sg = dt_in("sg_norm_g", [512])
    g_sbo = dt_in("sb_out_norm_g", [512])
    g_sgo = dt_in("sg_out_norm_g", [512])
    w_r = dt_in("w_router", [D, 36])
    b_r = dt_in("b_router", [36])
    y = nc.dram_tensor("y", [T, D], F32, kind="ExternalOutput").ap()
    xs = nc.dram_tensor("xs_scratch", [48 * 256, D], BF16, kind="Internal").ap()
    ys = nc.dram_tensor("ys_scratch", [48 * 256, D], F32, kind="Internal").ap()

    with ExitStack() as es:
        S = Sched(nc, es)
        sbt = lambda name, shape, dt: es.enter_context(nc.sbuf_tensor(name, shape, dt))
        R1 = sbt("R1", [P, 16384], F32)
        R2 = sbt("R2", [P, 12288], F32)
        R3 = sbt("R3", [P, 12288], F32)
        R4 = sbt("R4", [P, 4096], F32)
        ident = sbt("ident", [P, 128], BF16)
        identf = sbt("identf", [P, 128], F32)
        negU = sbt("negU", [P, 128], BF16)
        negones = sbt("negones", [P, 128], BF16)
        ones = sbt("ones", [P, 128], BF16)
        zeros = sbt("zeros", [P, 512], BF16)
        gc_attn = sbt("gc_attn", [P, 8], F32)
        gc_ffn = sbt("gc_ffn", [P, 8], F32)
        gc_sbo = sbt("gc_sbo", [P, 4], F32)
        gc_sgo = sbt("gc_sgo", [P, 4], F32)
        gb_sg = sbt("gb_sg", [P, 512], F32)
        gb_fin = sbt("gb_fin", [P, D], F32)
        brb = sbt("brb", [P, 36], F32)
        bT = sbt("bT", [P, 8], F32)
        WmT = sbt("WmT", [P, 8 * 128], BF16)
        wr_f = sbt("wr_f", [P, 8 * 36], F32)
        neghalf = sbt("neghalf", [P, 1], F32)
        stat = sbt("stat", [P, 128], F32)
        gu2 = sbt("gu2", [P, 1024], F32)
        gb_ffn = sbt("gb_ffn", [P, D], F32)
        Ustrict = sbt("Ustrict", [P, 128], BF16)
        thr = sbt("thr", [P, 48], F32)
        p2 = sbt("p2", [P, 1], F32)
        s1b = sbt("s1b", [P, NT * 32], BF16)
        s2b = sbt("s2b", [P, NT * 32], BF16)
        selb = sbt("selb", [P, NT * 32], BF16)
        wts = sbt("wts", [P, NT * 2], F32)
        slotf = sbt("slotf", [P, 2 * NT], F32)
        sloti = sbt("sloti", [P, 2 * NT], I32)
        eitf = sbt("eitf", [P, 48], F32)
        widx = sbt("widx", [P, 96], I32)
        rt = sbt("rt", [P, 576], F32)
        rt2 = sbt("rt2", [P, 416], F32)
        lgall = sbt("lgall", [P, NT * 36], F32)
        PS = es.enter_context(nc.psum_tensor("PS", [P, 4096], F32))
        pb = [PS[:, i * 512:(i + 1) * 512] for i in range(8)]
        PB = [Buf("pb%d" % i) for i in range(8)]

        cB = {n: Buf(n) for n in ["ident", "identf", "negU", "negones", "ones", "zeros", "gc_attn", "gc_ffn",
                                  "gc_sbo", "gc_sgo", "gb_sg", "gb_fin", "brb", "bT", "WmT", "wr_f", "neghalf", "gb_ffn", "Ustrict", "thr", "p2"]}

        def bf(ap):
            return ap.bitcast(BF16)

        S.op("pool", lambda e: e.memset(ident[:], 1.0), writes=[cB["ident"]])
        S.op("pool", lambda e: e.affine_select(out=ident[:], in_=ident[:], pattern=[[-1, 128]], compare_op=ALU.is_equal,
                                               fill=0.0, base=0, channel_multiplier=1), reads=[cB["ident"]], writes=[cB["ident"]])
        S.op("pool", lambda e: e.memset(negU[:], -1.0), writes=[cB["negU"]])
        S.op("pool", lambda e: e.affine_select(out=negU[:], in_=negU[:], pattern=[[-1, 128]], compare_op=ALU.is_ge,
                                               fill=0.0, base=0, channel_multiplier=1), reads=[cB["negU"]], writes=[cB["negU"]])
        S.op("pool", lambda e: e.memset(negones[:], -1.0), writes=[cB["negones"]])
        S.op("pool", lambda e: e.memset(ones[:], 1.0), writes=[cB["ones"]])
        S.op("pool", lambda e: e.memset(zeros[:], 0.0), writes=[cB["zeros"]])
        S.op("pool", lambda e: e.memset(neghalf[:], -0.5), writes=[cB["neghalf"]])
        S.op("pool", lambda e: e.memset(Ustrict[:], 1.0), writes=[cB["Ustrict"]])
        S.op("pool", lambda e: e.affine_select(out=Ustrict[:], in_=Ustrict[:], pattern=[[1, 128]], compare_op=ALU.is_gt,
                                               fill=0.0, base=0, channel_multiplier=-1), reads=[cB["Ustrict"]], writes=[cB["Ustrict"]])
        S.op("pool", lambda e: e.iota(thr[:], pattern=[[256, 48]], base=0, channel_multiplier=0, allow_small_or_imprecise_dtypes=True),
             writes=[cB["thr"]])
        S.op("pool", lambda e: e.iota(p2[:], pattern=[[0, 1]], base=0, channel_multiplier=2, allow_small_or_imprecise_dtypes=True),
             writes=[cB["p2"]])
        S.op("dve", lambda e: e.tensor_copy(out=identf[:], in_=ident[:]), reads=[cB["ident"]], writes=[cB["identf"]])
        with nc.allow_non_contiguous_dma(reason="tiny parameter vectors"):
            S.dma("sp", gc_attn[:], g_attn.rearrange("(c p) -> p c", p=P), cB["gc_attn"], writes=[cB["gc_attn"]])
            S.dma("sp", gc_ffn[:], g_ffn.rearrange("(c p) -> p c", p=P), cB["gc_ffn"], writes=[cB["gc_ffn"]])
            S.dma("sp", gc_sbo[:], g_sbo.rearrange("(c p) -> p c", p=P), cB["gc_sbo"], writes=[cB["gc_sbo"]])
            S.dma("sp", gc_sgo[:], g_sgo.rearrange("(c p) -> p c", p=P), cB["gc_sgo"], writes=[cB["gc_sgo"]])
            S.dma("sp", bT[:], b_sp.rearrange("h t -> t h"), cB["bT"], writes=[cB["bT"]])
        S.dma("sp", gb_sg[:], g_sg.partition_broadcast(P), cB["gb_sg"], writes=[cB["gb_sg"]])
        S.dma("sp", gb_fin[:], g_fin.partition_broadcast(P), cB["gb_fin"], writes=[cB["gb_fin"]])
        S.dma("sp", gb_ffn[:], g_ffn.partition_broadcast(P), cB["gb_ffn"], writes=[cB["gb_ffn"]])
        S.dma("sp", brb[:], b_r.partition_broadcast(P), cB["brb"], writes=[cB["brb"]])
        S.dma("sp", wr_f[:].rearrange("p (c n) -> p c n", c=8), w_r.rearrange("(c p) n -> p c n", p=P), cB["wr_f"], writes=[cB["wr_f"]])

        wsp_f = R4[:, 0:1024].rearrange("p (h s) -> p h s", h=8)
        wsp_b = bf(R4[:, 1024:1536])
        B_wspf = Buf("wspf")
        B_wspb = Buf("wspb")
        S.dma("sp", wsp_f, w_sp.rearrange("h t s -> t h s"), B_wspf, writes=[B_wspf])
        S.op("pool", lambda e: e.affine_select(out=wsp_b.rearrange("p (h s) -> p h s", h=8), in_=wsp_f,
                                               pattern=[[0, 8], [-1, 128]], compare_op=ALU.is_ge, fill=0.0, base=0,
                                               channel_multiplier=1), reads=[B_wspf], writes=[B_wspb])
        tpv = bf(pb[0][:])
        for h in range(8):
            S.op("pe", lambda e, h=h: e.transpose(out=tpv[:, h * 128:(h + 1) * 128], in_=wsp_b[:, h * 128:(h + 1) * 128],
                                                  identity=ident[:]), reads=[B_wspb, cB["ident"]], writes=[PB[0]])
        S.op("dve", lambda e: e.tensor_copy(out=WmT[:], in_=tpv), reads=[PB[0]], writes=[cB["WmT"]])

        win_sb = bf(R1[:, 0:10240]).rearrange("p (c n) -> p c n", c=8)
        B_win = [Buf("win%d" % i) for i in range(5)]
        w_in_v = w_in.rearrange("(c p) n -> p c n", p=P)
        for part in range(5):
            S.dma("pool", win_sb[:, :, part * 512:(part + 1) * 512], w_in_v[:, :, part * 512:(part + 1) * 512],
                  B_win[part], writes=[B_win[part]])

        wout_sb = bf(R2[:, 8192:12288]).rearrange("p (c n) -> p c n", c=8)
        B_wout = Buf("wout")
        S.dma("pool", wout_sb, w_out.rearrange("(c p) n -> p c n", p=P), B_wout, writes=[B_wout])
        hnT = bf(R2[:, 0:8192]).rearrange("p (c n) -> p c n", c=8)
        B_hnT = [Buf("hnT%d" % i) for i in range(NT)]
        xring = [R1[:, 10240 + k * 1024:10240 + (k + 1) * 1024] for k in range(2)]
        B_xr = [Buf("xr%d" % k) for k in range(2)]
        xnring = [bf(R1[:, 12288 + k * 512:12288 + (k + 1) * 512]) for k in range(2)]
        B_xn = [Buf("xn%d" % k) for k in range(2)]
        junk = bf(R1[:, 15872:16384])
        B_junk = Buf("junk")
        B_stat = Buf("stat")
        B_st1 = [Buf("st1_%d" % i) for i in range(NT)]
        x_t = x.rearrange("(i p) d -> i p d", p=P)
        y_t = y.rearrange("(i p) d -> i p d", p=P)

        def rstd_from_ss(col, n, buf=None):
            buf = B_stat if buf is None else buf
            S.op("dve", lambda e: e.tensor_scalar(out=stat[:, col + 1:col + 2], in0=stat[:, col:col + 1], scalar1=1.0 / n,
                                                  scalar2=EPS, op0=ALU.mult, op1=ALU.add), reads=[buf], writes=[buf])
            S.op("pool", lambda e: e.tensor_tensor(out=stat[:, col + 1:col + 2], in0=stat[:, col + 1:col + 2], in1=neghalf[:],
                                                   op=ALU.pow), reads=[buf, cB["neghalf"]], writes=[buf])

        def rstd_act(col, n, buf):
            S.op("dve", lambda e: e.tensor_scalar(out=stat[:, col + 1:col + 2], in0=stat[:, col:col + 1], scalar1=1.0 / n,
                                                  scalar2=EPS, op0=ALU.mult, op1=ALU.add), reads=[buf], writes=[buf])
            S.op("act", lambda e: e.activation(out=stat[:, col + 1:col + 2], in_=stat[:, col + 1:col + 2], func=AF.Sqrt), reads=[buf], writes=[buf])
            S.op("dve", lambda e: e.reciprocal(out=stat[:, col + 1:col + 2], in_=stat[:, col + 1:col + 2]), reads=[buf], writes=[buf])

        for i in range(NT):
            k = i % 2
            S.dma("sp", xring[k], x_t[i], B_xr[k], writes=[B_xr[k]])
            c1 = 64 + 2 * i
            S.op("act", lambda e, k=k, c1=c1: e.activation(out=xnring[k], in_=xring[k], func=AF.Square, accum_out=stat[:, c1:c1 + 1]),
                 reads=[B_xr[k]], writes=[B_xn[k], B_st1[i]])
            rstd_act(c1, D, B_st1[i])
            S.op("dve", lambda e, k=k, c1=c1: e.tensor_scalar(out=xnring[k], in0=xring[k], scalar1=stat[:, c1 + 1:c1 + 2], scalar2=None,
                                                              op0=ALU.mult), reads=[B_xr[k], B_st1[i]], writes=[B_xn[k]])
            tb = k
            tpk = bf(pb[tb][:])
            S.ops("pe", [lambda e, c=c, k=k, tpk=tpk: e.transpose(out=tpk[:, c * 128:(c + 1) * 128],
                                                                  in_=xnring[k][:, c * 128:(c + 1) * 128], identity=ident[:])
                         for c in range(8)], reads=[B_xn[k], cB["ident"]], writes=[PB[tb]])
            S.op("dve", lambda e, i=i, tpk=tpk: e.tensor_tensor(out=hnT[:, :, i * 128:(i + 1) * 128],
                                                                in0=tpk.rearrange("p (c n) -> p c n", c=8),
                                                                in1=gc_attn[:].unsqueeze(2).to_broadcast([P, 8, 128]), op=ALU.mult),
                 reads=[PB[tb], cB["gc_attn"]], writes=[B_hnT[i]])

        qT = bf(R3[:, 0:4096]).rearrange("p (j n) -> p j n", j=4)
        kT = bf(R3[:, 4096:8192]).rearrange("p (j n) -> p j n", j=4)
        vS = bf(R3[:, 8192:12288]).rearrange("p (i n) -> p i n", i=NT)
        B_q = [[Buf("q%d_%d" % (j, t)) for t in range(4)] for j in range(4)]
        B_k = [[Buf("k%d_%d" % (j, t)) for t in range(4)] for j in range(4)]
        B_v = [Buf("v%d" % i) for i in range(NT)]
        cnt = 0
        for part, dstT, dstB in ((0, qT, B_q), (1, kT, B_k)):
            for j in range(4):
                for tq in range(4):
                    bank = 2 + (cnt % 2)
                    cnt += 1
                    S.ops("pe", [lambda e, c=c, j=j, tq=tq, part=part, bank=bank: e.matmul(
                        pb[bank][:, :], lhsT=win_sb[:, c, part * 512 + j * 128:part * 512 + (j + 1) * 128],
                        rhs=hnT[:, c, tq * 512:(tq + 1) * 512], start=(c == 0), stop=(c == 7)) for c in range(8)],
                        reads=[B_win[part]] + B_hnT[tq * 4:(tq + 1) * 4], writes=[PB[bank]])
                    if part == 0:
                        S.op("act", lambda e, j=j, tq=tq, bank=bank: e.activation(out=qT[:, j, tq * 512:(tq + 1) * 512], in_=pb[bank][:, :],
                                                                                func=AF.Copy, scale=0.125),
                             reads=[PB[bank]], writes=[dstB[j][tq]])
                    else:
                        S.op("dve", lambda e, j=j, tq=tq, bank=bank: e.tensor_copy(out=kT[:, j, tq * 512:(tq + 1) * 512], in_=pb[bank][:, :]),
                             reads=[PB[bank]], writes=[dstB[j][tq]])
        for i in range(NT):
            bank = 4 + (i % 2)
            S.ops("pe", [lambda e, c=c, i=i, bank=bank: e.matmul(pb[bank][:, :], lhsT=hnT[:, c, i * 128:(i + 1) * 128],
                                                                 rhs=win_sb[:, c, 1024:1536], start=(c == 0), stop=(c == 7))
                         for c in range(8)], reads=[B_win[2], B_hnT[i]], writes=[PB[bank]])
            eng = "act" if i % 2 == 0 else "dve"
            if eng == "act":
                S.op("act", lambda e, i=i, bank=bank: e.activation(out=vS[:, i, :], in_=pb[bank][:, :], func=AF.Copy),
                     reads=[PB[bank]], writes=[B_v[i]])
            else:
                S.op("dve", lambda e, i=i, bank=bank: e.tensor_copy(out=vS[:, i, :], in_=pb[bank][:, :]),
                     reads=[PB[bank]], writes=[B_v[i]])

        mixT_sg = bf(R4[:, 0:4096]).rearrange("p (c n) -> p c n", c=4)
        B_msg = [Buf("msg%d" % i, after=[B_wspf, B_wspb]) for i in range(NT)]
        gu_t = [gu2[:, k * 512:(k + 1) * 512] for k in range(2)]
        gv_t = [R1[:, 13312 + k * 1536:13824 + k * 1536] for k in range(2)]
        m1_t = [R1[:, 13824 + k * 1536:14336 + k * 1536] for k in range(2)]
        vs_t = [bf(R1[:, 14336 + k * 1536:14592 + k * 1536]) for k in range(2)]
        sgn_t = [bf(R1[:, 14592 + k * 1536:14848 + k * 1536]) for k in range(2)]
        B_gu2 = [Buf("gu%d" % k) for k in range(2)]
        B_gv2 = [Buf("gv%d" % k) for k in range(2)]
        B_m12 = [Buf("m1_%d" % k) for k in range(2)]
        B_vs2 = [Buf("vs%d" % k) for k in range(2)]
        B_sgn2 = [Buf("sgn%d" % k) for k in range(2)]
        B_st3 = [Buf("st3_%d" % i) for i in range(NT)]
        B_gu, B_gv, B_vgn, B_m1, B_sg, B_sgn = B_gu2[0], B_gv2[0], B_vs2[0], B_m12[0], B_sgn2[0], B_sgn2[1]
        extra_R1 = [B_gv2[1], B_m12[1], B_vs2[1]]

        def sgA(i):
            k = i % 2
            bu, bv = (2, 3) if k == 0 else (5, 6)
            c3 = 4 * i
            for (bank, col0, dst, dB) in ((bu, 1536, gu_t[k], B_gu2[k]), (bv, 2048, gv_t[k], B_gv2[k])):
                S.ops("pe", [lambda e, c=c, i=i, bank=bank, col0=col0: e.matmul(pb[bank][:, :], lhsT=hnT[:, c, i * 128:(i + 1) * 128],
                                                                                rhs=win_sb[:, c, col0:col0 + 512], start=(c == 0), stop=(c == 7))
                             for c in range(8)], reads=[B_win[3], B_win[4], B_hnT[i]], writes=[PB[bank]])
                S.op("act", lambda e, bank=bank, dst=dst: e.activation(out=dst, in_=pb[bank][:, :], func=AF.Gelu_apprx_tanh),
                     reads=[PB[bank]], writes=[dB])
            S.op("act", lambda e, k=k, c3=c3: e.activation(out=vs_t[k], in_=gv_t[k], func=AF.Square, accum_out=stat[:, c3:c3 + 1]),
                 reads=[B_gv2[k]], writes=[B_vs2[k], B_st3[i]])
            rstd_from_ss(c3, 512, B_st3[i])
            S.op("dve", lambda e, k=k, c3=c3: e.tensor_scalar(out=vs_t[k], in0=gv_t[k], scalar1=stat[:, c3 + 1:c3 + 2], scalar2=None, op0=ALU.mult),
                 reads=[B_gv2[k], B_st3[i]], writes=[B_vs2[k]])

        def sgB(i):
            k = i % 2
            bm = 4 if k == 0 else 7
            c3 = 4 * i + 2
            S.ops("pe", [lambda e, h=h, k=k, bm=bm: e.matmul(pb[bm][:, h * 64:(h + 1) * 64], lhsT=WmT[:, h * 128:(h + 1) * 128],
                                                             rhs=vs_t[k][:, h * 64:(h + 1) * 64], start=True, stop=True) for h in range(8)],
                  reads=[B_vs2[k], cB["WmT"]], writes=[PB[bm]])
            S.op("dve", lambda e, k=k, bm=bm: e.tensor_tensor(out=m1_t[k], in0=pb[bm][:, :], in1=gb_sg[:], op=ALU.mult),
                 reads=[PB[bm], cB["gb_sg"]], writes=[B_m12[k]])
            S.op("dve", lambda e, k=k: e.tensor_tensor(out=m1_t[k].rearrange("p (h d) -> p h d", h=8), in0=m1_t[k].rearrange("p (h d) -> p h d", h=8),
                                                       in1=bT[:].unsqueeze(2).to_broadcast([P, 8, 64]), op=ALU.add),
                 reads=[B_m12[k], cB["bT"]], writes=[B_m12[k]])
            S.op("dve", lambda e, k=k: e.tensor_tensor(out=m1_t[k], in0=m1_t[k], in1=gu_t[k], op=ALU.mult), reads=[B_m12[k], B_gu2[k]], writes=[B_m12[k]])
            S.op("act", lambda e, k=k, c3=c3: e.activation(out=sgn_t[k], in_=m1_t[k], func=AF.Square, accum_out=stat[:, c3:c3 + 1]),
                 reads=[B_m12[k]], writes=[B_sgn2[k], B_st3[i]])
            rstd_from_ss(c3, 512, B_st3[i])
            S.op("dve", lambda e, k=k, c3=c3: e.tensor_scalar(out=sgn_t[k], in0=m1_t[k], scalar1=stat[:, c3 + 1:c3 + 2], scalar2=None, op0=ALU.mult),
                 reads=[B_m12[k], B_st3[i]], writes=[B_sgn2[k]])

        def sgC(i):
            k = i % 2
            tpk = bf(pb[k][:])
            S.ops("pe", [lambda e, c=c, tpk=tpk, k=k: e.transpose(out=tpk[:, c * 128:(c + 1) * 128], in_=sgn_t[k][:, c * 128:(c + 1) * 128],
                                                                  identity=ident[:]) for c in range(4)],
                  reads=[B_sgn2[k], cB["ident"]], writes=[PB[k]])
            S.op("dve", lambda e, i=i, tpk=tpk: e.tensor_tensor(out=mixT_sg[:, :, i * 128:(i + 1) * 128],
                                                                in0=tpk[:, 0:512].rearrange("p (c n) -> p c n", c=4),
                                                                in1=gc_sgo[:].unsqueeze(2).to_broadcast([P, 4, 128]), op=ALU.mult),
                 reads=[PB[k], cB["gc_sgo"]], writes=[B_msg[i]])

        sgA(0)
        sgA(1)
        sgB(0)
        for i in range(NT):
            if i + 2 < NT:
                sgA(i + 2)
            if i + 1 < NT:
                sgB(i + 1)
            sgC(i)

        old_R1 = B_win + B_xr + B_xn + [B_gu, B_gv, B_vgn, B_m1, B_sg, B_sgn, B_junk] + extra_R1
        oT = R1[:, 0:8192].rearrange("p (j n) -> p j n", j=4)
        B_oT = [[Buf("oT%d_%d" % (j, qc), after=old_R1) for qc in range(4)] for j in range(4)]
        o = 8192
        e2 = R1[:, o:o + 1024].rearrange("p (h n) -> p h n", h=2)
        sp2 = [bf(R1[:, o + 1024 + k * 512:o + 1536 + k * 512]).rearrange("p (h n) -> p h n", h=2) for k in range(2)]
        a2 = [bf(R1[:, o + 2048 + k * 512:o + 2560 + k * 512]).rearrange("p (h n) -> p h n", h=2) for k in range(2)]
        S_t = [[bf(R1[:, o + 3072 + (h * 2 + k) * 256:o + 3328 + (h * 2 + k) * 256]) for k in range(2)] for h in range(2)]
        B_e = [Buf("e2", after=old_R1)]
        B_sp = [Buf("sp2_%d" % k, after=old_R1) for k in range(2)]
        B_a = [Buf("a2_%d" % k, after=old_R1) for k in range(2)]
        B_S = [[Buf("S%d_%d" % (h, k), after=old_R1) for k in range(2)] for h in range(2)]

        units = []
        gi = 0
        for j in range(4):
            for qc in range(4):
                kbs = list(range(4 * qc + 3, -1, -1))
                for ui, kb in enumerate(kbs):
                    r = kb - 4 * qc
                    c0 = 128 * r if r >= 0 else 0
                    units.append(dict(j=j, qc=qc, kb=kb, r=r, c0=c0, N=512 - c0, ui=ui, first=(ui == 0),
                                      last=(ui == len(kbs) - 1), gi=gi))
                gi += 1
        nU = len(units)

        def qk_mm(e, out_ap, U, h, start, stop):
            b = 64 * h
            j, kb, qc, c0, N = U["j"], U["kb"], U["qc"], U["c0"], U["N"]
            q0 = qc * 512 + c0
            return e.matmul(out_ap, lhsT=kT[b:b + 64, j, kb * 128:(kb + 1) * 128], rhs=qT[b:b + 64, j, q0:q0 + N],
                            start=start, stop=stop)

        def zbank(u, h):
            return 2 * (u % 2) + h

        def stA(u):
            U = units[u]
            for h in range(2):
                zb = zbank(u, h)
                S.op("pe", lambda e, U=U, h=h, zb=zb: qk_mm(e, pb[zb][:, 0:U["N"]], U, h, True, True),
                     reads=[B_k[U["j"]][U["kb"] // 4], B_q[U["j"]][U["qc"]]], writes=[PB[zb]])

        def stB(u):
            U = units[u]
            N = U["N"]
            z0 = zbank(u, 0)
            zin = PS[:, z0 * 512:(z0 + 2) * 512].rearrange("p (h n) -> p h n", h=2)[:, :, 0:N]
            S.op("act", lambda e, N=N, zin=zin: e.activation(out=e2[:, :, 0:N], in_=zin, func=AF.Exp),
                 reads=[PB[z0], PB[z0 + 1]], writes=[B_e[0]])
            S.op("act", lambda e, N=N, u=u: e.activation(out=sp2[u % 2][:, :, 0:N], in_=e2[:, :, 0:N], func=AF.Ln, bias=1.0),
                 reads=[B_e[0]], writes=[B_sp[u % 2]])

        def mask_tile(ap, bufs):
            S.op("pool", lambda e: e.affine_select(out=ap, in_=ap, pattern=[[1, 128]], compare_op=ALU.is_gt, fill=0.0, base=0,
                                                   channel_multiplier=-1), reads=bufs, writes=bufs)

        def stC(u):
            U = units[u]
            if U["r"] >= 0:
                for h in range(2):
                    mask_tile(sp2[u % 2][:, h, 0:128], [B_sp[u % 2]])

        def stD(u):
            U = units[u]
            N, c0, ui = U["N"], U["c0"], U["ui"]
            cur, nxt = ui % 2, 1 - (ui % 2)
            for h in range(2):
                if U["first"]:
                    S.op("dve", lambda e, h=h: e.memset(S_t[h][0][:, :], 0.0), writes=[B_S[h][0]])
                    S.op("dve", lambda e, h=h: e.memset(S_t[h][1][:, :], 0.0), writes=[B_S[h][1]])
                S.ops("pe", [
                    lambda e, h=h, N=N, u=u: e.matmul(pb[4 + h][:, 0:N], lhsT=negU[:], rhs=sp2[u % 2][:, h, 0:N], start=True, stop=False),
                    lambda e, h=h, N=N, c0=c0, cur=cur: e.matmul(pb[4 + h][:, 0:N], lhsT=negones[:], rhs=S_t[h][cur][:, c0:512], start=False, stop=False),
                    lambda e, h=h, U=U: qk_mm(e, pb[4 + h][:, 0:U["N"]], U, h, False, True)],
                    reads=[B_sp[u % 2], B_S[h][cur], B_k[U["j"]][U["kb"] // 4], B_q[U["j"]][U["qc"]], cB["negU"], cB["negones"]],
                    writes=[PB[4 + h]])
                if not U["last"]:
                    S.op("dve", lambda e, h=h, N=N, c0=c0, cur=cur, nxt=nxt, u=u: e.tensor_tensor(
                        out=S_t[h][nxt][:, c0:512], in0=S_t[h][cur][:, c0:512], in1=sp2[u % 2][:, h, 0:N], op=ALU.add),
                        reads=[B_S[h][cur], B_sp[u % 2]], writes=[B_S[h][nxt]])

        def stE(u):
            U = units[u]
            N = U["N"]
            ain = PS[:, 4 * 512:6 * 512].rearrange("p (h n) -> p h n", h=2)[:, :, 0:N]
            S.op("act", lambda e, N=N, u=u, ain=ain: e.activation(out=a2[u % 2][:, :, 0:N], in_=ain, func=AF.Exp),
                 reads=[PB[4], PB[5]], writes=[B_a[u % 2]])

        def stF(u):
            U = units[u]
            if U["r"] >= 0:
                for h in range(2):
                    mask_tile(a2[u % 2][:, h, 0:128], [B_a[u % 2]])

        def stG(u):
            U = units[u]
            j, kb, qc, c0, N = U["j"], U["kb"], U["qc"], U["c0"], U["N"]
            ob = 6 + (U["gi"] % 2)
            if U["first"]:
                for h in range(2):
                    S.op("pe", lambda e, ob=ob, h=h: e.matmul(pb[ob][64 * h:64 * h + 64, :], lhsT=zeros[:, 0:64], rhs=zeros[:, :], start=True, stop=False),
                         reads=[cB["zeros"]], writes=[PB[ob]])
            for h in range(2):
                hh = 2 * j + h
                S.op("pe", lambda e, h=h, hh=hh, ob=ob, kb=kb, c0=c0, N=N, u=u, U=U: e.matmul(
                    pb[ob][64 * h:64 * h + 64, c0:512], lhsT=vS[:, kb, hh * 64:(hh + 1) * 64], rhs=a2[u % 2][:, h, 0:N],
                    start=False, stop=U["last"]), reads=[B_a[u % 2], B_v[kb]], writes=[PB[ob]])
            if U["last"]:
                S.op("dve", lambda e, j=j, qc=qc, ob=ob: e.tensor_copy(out=oT[:, j, qc * 512:(qc + 1) * 512], in_=pb[ob][:, :]),
                     reads=[PB[ob]], writes=[B_oT[j][qc]])

        stA(0)
        stA(1)
        stB(0)
        stC(0)
        for u in range(nU):
            if u + 2 < nU:
                stA(u + 2)
            stD(u)
            if u >= 1:
                stG(u - 1)
            if u + 1 < nU:
                stB(u + 1)
                stC(u + 1)
            stE(u)
            stF(u)
        stG(nU - 1)

        allq = [b for row in B_q for b in row]
        mixT_sb = bf(R3[:, 0:4096]).rearrange("p (c n) -> p c n", c=4)
        B_msb = [Buf("msb%d" % t, after=allq) for t in range(4)]
        att_work = B_e + B_sp + B_a + [b for l in B_S for b in l]
        sq_t = [bf(R1[:, 12288 + k * 256:12544 + k * 256]) for k in range(4)]
        B_sq = [Buf("sq%d" % k, after=old_R1) for k in range(4)]
        rb = R1[:, 13312:13824]
        B_rb = Buf("rb", after=old_R1)
        for tq in range(4):
            for j in range(4):
                S.op("act", lambda e, j=j, tq=tq: e.activation(out=sq_t[j][:, :], in_=oT[:, j, tq * 512:(tq + 1) * 512], func=AF.Square),
                     reads=[B_oT[j][tq]], writes=[B_sq[j]])
            S.ops("pe", [lambda e, j=j: e.matmul(pb[0][:, :], lhsT=ones[:], rhs=sq_t[j][:, :], start=(j == 0), stop=(j == 3))
                         for j in range(4)], reads=B_sq + [cB["ones"]], writes=[PB[0]])
            S.op("dve", lambda e: e.tensor_scalar(out=rb, in0=pb[0][:, :], scalar1=1.0 / 512, scalar2=EPS, op0=ALU.mult, op1=ALU.add),
                 reads=[PB[0]], writes=[B_rb])
            S.op("act", lambda e: e.activation(out=rb, in_=rb, func=AF.Sqrt), reads=[B_rb], writes=[B_rb])
            S.op("dve", lambda e: e.reciprocal(out=rb, in_=rb), reads=[B_rb], writes=[B_rb])
            for j in range(4):
                S.op("dve", lambda e, j=j, tq=tq: e.scalar_tensor_tensor(out=mixT_sb[:, j, tq * 512:(tq + 1) * 512],
                                                                         in0=oT[:, j, tq * 512:(tq + 1) * 512], scalar=gc_sbo[:, j:j + 1],
                                                                         in1=rb, op0=ALU.mult, op1=ALU.mult),
                     reads=[B_oT[j][tq], B_rb, cB["gc_sbo"]], writes=[B_msb[tq]])

        h1 = R1[:, :].rearrange("p (i d) -> p i d", i=NT)
        all_R1_p4 = [b for row in B_oT for b in row] + att_work + B_sq + [B_rb]
        B_h1 = [Buf("h1_%d" % i, after=all_R1_p4) for i in range(NT)]
        xr2 = [R2[:, k * 1024:(k + 1) * 1024] for k in range(2)]
        B_xr2 = [Buf("xr2_%d" % k, after=B_hnT) for k in range(2)]
        for i in range(NT):
            k = i % 2
            S.dma("sp", xr2[k], x_t[i], B_xr2[k], writes=[B_xr2[k]])
            for half in range(2):
                bank = (2 * i + half) % 4
                S.ops("pe", [lambda e, c=c, i=i, half=half, bank=bank: e.matmul(
                    pb[bank][:, :], lhsT=(mixT_sb[:, c, i * 128:(i + 1) * 128] if c < 4 else mixT_sg[:, c - 4, i * 128:(i + 1) * 128]),
                    rhs=wout_sb[:, c, half * 512:(half + 1) * 512], start=(c == 0), stop=(c == 7)) for c in range(8)],
                    reads=[B_msb[i // 4], B_msg[i], B_wout], writes=[PB[bank]])
                S.op("dve", lambda e, i=i, half=half, bank=bank, k=k: e.tensor_tensor(
                    out=h1[:, i, half * 512:(half + 1) * 512], in0=pb[bank][:, :], in1=xr2[k][:, half * 512:(half + 1) * 512], op=ALU.add),
                    reads=[PB[bank], B_xr2[k]], writes=[B_h1[i]])

        def finish(src_tiles, src_bufs):
            toks = []
            for i in range(NT):
                toks.append(S.dma("sp", y_t[i], src_tiles(i), src_bufs[i], reads=[src_bufs[i]]))
            for t in toks:
                S.wait_tok("sp", t)

        if stage == "h1":
            finish(lambda i: h1[:, i, :], B_h1)
            return nc

        NIT = 48
        NRUN = 47
        NSLOT = NIT * 256
        hn2b = bf(R2[:, 0:8192]).rearrange("p (i d) -> p i d", i=NT)
        B_hn2b = [Buf("hn2b%d" % i, after=B_xr2 + B_hnT) for i in range(NT)]
        B_rt = Buf("rt")
        B_rs = Buf("routing_state")
        s1b3 = s1b[:].rearrange("p (i n) -> p i n", i=NT)
        s2b3 = s2b[:].rearrange("p (i n) -> p i n", i=NT)
        selb3 = selb[:].rearrange("p (i n) -> p i n", i=NT)
        wts3 = wts[:].rearrange("p (i n) -> p i n", i=NT)

        def dve(fn, reads, writes):
            S.op("dve", fn, reads=reads, writes=writes)

        hn2f2 = [R2[:, 8192 + k * 1024:9216 + k * 1024] for k in range(2)]
        hn2Tf2 = [R2[:, 10240 + k * 1024:11264 + k * 1024].rearrange("p (c n) -> p c n", c=8) for k in range(2)]
        B_hn2f2 = [Buf("hn2f_%d" % k, after=[B_wout]) for k in range(2)]
        B_hn2Tf2 = [Buf("hn2Tf_%d" % k, after=[B_wout]) for k in range(2)]
        B_st6 = [Buf("st6_%d" % i) for i in range(NT)]
        B_lg = [Buf("lg%d" % i) for i in range(NT)]
        lg3 = lgall[:].rearrange("p (i n) -> p i n", i=NT)
        for i in range(NT):
            k = i % 2
            tb0, tb1, rb_ = (0, 1, 2) if k == 0 else (4, 5, 6)
            c0s = 16 + 2 * i
            S.op("act", lambda e, i=i, c0s=c0s: e.activation(out=hn2b[:, i, :], in_=h1[:, i, :], func=AF.Square, accum_out=stat[:, c0s:c0s + 1]),
                 reads=[B_h1[i]], writes=[B_hn2b[i], B_st6[i]])
            rstd_from_ss(c0s, D, B_st6[i])
            dve(lambda e, i=i, k=k, c0s=c0s: e.scalar_tensor_tensor(out=hn2f2[k], in0=h1[:, i, :], scalar=stat[:, c0s + 1:c0s + 2], in1=gb_ffn[:],
                                                                    op0=ALU.mult, op1=ALU.mult),
                [B_h1[i], B_st6[i], cB["gb_ffn"]], [B_hn2f2[k]])
            S.op("act", lambda e, i=i, k=k: e.activation(out=hn2b[:, i, :], in_=hn2f2[k], func=AF.Copy), reads=[B_hn2f2[k]], writes=[B_hn2b[i]])
            for hb, tb in ((0, tb0), (1, tb1)):
                S.ops("pe", [lambda e, c=c, tb=tb, k=k: e.transpose(out=pb[tb][:, (c % 4) * 128:(c % 4 + 1) * 128],
                                                                    in_=hn2f2[k][:, c * 128:(c + 1) * 128], identity=identf[:])
                             for c in range(4 * hb, 4 * hb + 4)], reads=[B_hn2f2[k], cB["identf"]], writes=[PB[tb]])
            S.op("act", lambda e, k=k, tb0=tb0: e.activation(out=hn2Tf2[k][:, 0:4, :], in_=pb[tb0][:, :].rearrange("p (c n) -> p c n", c=4), func=AF.Copy),
                 reads=[PB[tb0]], writes=[B_hn2Tf2[k]])
            dve(lambda e, k=k, tb1=tb1: e.tensor_copy(out=hn2Tf2[k][:, 4:8, :], in_=pb[tb1][:, :].rearrange("p (c n) -> p c n", c=4)),
                [PB[tb1]], [B_hn2Tf2[k]])
            S.ops("pe", [lambda e, c=c, k=k, rb_=rb_: e.matmul(pb[rb_][:, 0:36], lhsT=hn2Tf2[k][:, c, :], rhs=wr_f[:, c * 36:(c + 1) * 36],
                                                               start=(c == 0), stop=(c == 7)) for c in range(8)],
                  reads=[B_hn2Tf2[k], cB["wr_f"]], writes=[PB[rb_]])
            dve(lambda e, i=i, rb_=rb_: e.tensor_tensor(out=lg3[:, i, :], in0=pb[rb_][:, 0:36], in1=brb[:], op=ALU.add),
                [PB[rb_], cB["brb"]], [B_lg[i]])

        def rtv(c0, n):
            return rt[:, c0:c0 + n]
        gl = lg3[:, :, 0:4]
        ell = lg3[:, :, 4:36].rearrange("p i (g k) -> p i g k", g=4)
        gmax, gsum, gw = rtv(0, 16), rtv(16, 16), rtv(32, 16)
        m1c, m2c, dlt, ed, w1, w2 = rtv(48, 16), rtv(64, 16), rtv(80, 16), rtv(96, 16), rtv(112, 16), rtv(128, 16)
        goh = rtv(144, 64).rearrange("p (i g) -> p i g", i=NT)
        gex = rtv(208, 64).rearrange("p (i g) -> p i g", i=NT)
        el = rtv(320, 128).rearrange("p (i k) -> p i k", i=NT)
        oh1 = rtv(448, 128).rearrange("p (i k) -> p i k", i=NT)
        el2 = rt2[:, 0:128].rearrange("p (i k) -> p i k", i=NT)
        oh2 = rt2[:, 128:256].rearrange("p (i k) -> p i k", i=NT)
        tmp4 = R4[:, 0:512].rearrange("p (i g k) -> p i g k", i=NT, g=4)
        B_r4 = Buf("r4tmp", after=B_msg)
        R_ = [B_rt]
        LG = B_lg
        dve(lambda e: e.tensor_reduce(out=gmax, in_=gl, axis=AX.X, op=ALU.max), LG, R_)
        dve(lambda e: e.tensor_tensor(out=goh, in0=gl, in1=gmax.unsqueeze(2).to_broadcast([P, NT, 4]), op=ALU.is_equal), LG + R_, R_)
        dve(lambda e: e.tensor_tensor(out=gex, in0=gl, in1=gmax.unsqueeze(2).to_broadcast([P, NT, 4]), op=ALU.subtract), LG + R_, R_)
        S.op("act", lambda e: e.activation(out=gex, in_=gex, func=AF.Exp), reads=R_, writes=R_)
        dve(lambda e: e.tensor_reduce(out=gsum, in_=gex, axis=AX.X, op=ALU.add), R_, R_)
        dve(lambda e: e.reciprocal(out=gw, in_=gsum), R_, R_)
        dve(lambda e: e.tensor_tensor(out=tmp4, in0=ell, in1=goh.unsqueeze(3).to_broadcast([P, NT, 4, 8]), op=ALU.mult), LG + R_, [B_r4])
        dve(lambda e: e.tensor_reduce(out=el, in_=tmp4.rearrange("p i g k -> p i k g"), axis=AX.X, op=ALU.add), [B_r4], R_)
        dve(lambda e: e.tensor_reduce(out=m1c, in_=el, axis=AX.X, op=ALU.max), R_, R_)
        dve(lambda e: e.tensor_tensor(out=oh1, in0=el, in1=m1c.unsqueeze(2).to_broadcast([P, NT, 8]), op=ALU.is_equal), R_, R_)
        dve(lambda e: e.scalar_tensor_tensor(out=rt2[:, 0:128], in0=rtv(448, 128), scalar=-1e30, in1=rtv(320, 128), op0=ALU.mult, op1=ALU.add), R_, R_)
        dve(lambda e: e.tensor_reduce(out=m2c, in_=el2, axis=AX.X, op=ALU.max), R_, R_)
        dve(lambda e: e.tensor_tensor(out=oh2, in0=el2, in1=m2c.unsqueeze(2).to_broadcast([P, NT, 8]), op=ALU.is_equal), R_, R_)
        dve(lambda e: e.tensor_tensor(out=dlt, in0=m2c, in1=m1c, op=ALU.subtract), R_, R_)
        S.op("act", lambda e: e.activation(out=ed, in_=dlt, func=AF.Exp), reads=R_, writes=R_)
        dve(lambda e: e.tensor_scalar(out=w1, in0=ed, scalar1=1.0, scalar2=None, op0=ALU.add), R_, R_)
        dve(lambda e: e.reciprocal(out=w1, in_=w1), R_, R_)
        dve(lambda e: e.tensor_tensor(out=w2, in0=ed, in1=w1, op=ALU.mult), R_, R_)
        dve(lambda e: e.tensor_tensor(out=wts3[:, :, 0], in0=w1, in1=gw, op=ALU.mult), R_, [B_rs])
        dve(lambda e: e.tensor_tensor(out=wts3[:, :, 1], in0=w2, in1=gw, op=ALU.mult), R_, [B_rs])
        s1b4 = s1b[:].rearrange("p (i g k) -> p i g k", i=NT, g=4)
        s2b4 = s2b[:].rearrange("p (i g k) -> p i g k", i=NT, g=4)
        dve(lambda e: e.tensor_tensor(out=s1b4, in0=goh.unsqueeze(3).to_broadcast([P, NT, 4, 8]), in1=oh1.unsqueeze(2).to_broadcast([P, NT, 4, 8]),
                                      op=ALU.mult), R_, [B_rs])
        dve(lambda e: e.tensor_tensor(out=s2b4, in0=goh.unsqueeze(3).to_broadcast([P, NT, 4, 8]), in1=oh2.unsqueeze(2).to_broadcast([P, NT, 4, 8]),
                                      op=ALU.mult), R_, [B_rs])
        dve(lambda e: e.tensor_tensor(out=selb[:], in0=s1b[:], in1=s2b[:], op=ALU.add), [B_rs], [B_rs])

        for i in range(NT):
            fns = [lambda e, i=i: e.matmul(pb[3][:, i * 32:(i + 1) * 32], lhsT=Ustrict[:], rhs=selb3[:, i, :], start=True, stop=(i == 0))]
            for i2 in range(i):
                fns.append(lambda e, i=i, i2=i2: e.matmul(pb[3][:, i * 32:(i + 1) * 32], lhsT=ones[:], rhs=selb3[:, i2, :],
                                                          start=False, stop=(i2 == i - 1)))
            S.ops("pe", fns, reads=[B_rs, cB["Ustrict"], cB["ones"]], writes=[PB[3]])
        S.ops("pe", [lambda e, i=i: e.matmul(pb[2][:, 0:32], lhsT=ones[:], rhs=selb3[:, i, :], start=(i == 0), stop=(i == NT - 1))
                     for i in range(NT)], reads=[B_rs, cB["ones"]], writes=[PB[2]])
        MAGIC = 12582912.0
        pcA, pcB, pc, incl, off = rt2[:, 256:288], rt2[:, 288:320], rt2[:, 320:352], rt2[:, 352:384], rt2[:, 384:416]
        R_ = [B_rt]
        dve(lambda e: e.tensor_scalar(out=pcA, in0=pb[2][:, 0:32], scalar1=1.0 / 256, scalar2=(255.0 / 256 - 0.5 + 1.0 / 512),
                                      op0=ALU.mult, op1=ALU.add), [PB[2]], R_)
        dve(lambda e: e.tensor_scalar(out=pcB, in0=pcA, scalar1=MAGIC, scalar2=None, op0=ALU.add), R_, R_)
        dve(lambda e: e.tensor_scalar(out=pcA, in0=pcB, scalar1=-MAGIC, scalar2=None, op0=ALU.add), R_, R_)
        dve(lambda e: e.tensor_scalar(out=pc, in0=pcA, scalar1=256.0, scalar2=None, op0=ALU.mult), R_, R_)
        dve(lambda e: e.tensor_copy(out=pcA, in_=pc), R_, R_)
        src, dst = pcA, pcB
        for dd in (1, 2, 4, 8, 16):
            dve(lambda e, src=src, dst=dst, dd=dd: e.tensor_copy(out=dst[:, 0:dd], in_=src[:, 0:dd]), R_, R_)
            dve(lambda e, src=src, dst=dst, dd=dd: e.tensor_tensor(out=dst[:, dd:32], in0=src[:, dd:32], in1=src[:, 0:32 - dd], op=ALU.add), R_, R_)
            src, dst = dst, src
        dve(lambda e, src=src: e.tensor_copy(out=incl, in_=src), R_, R_)
        dve(lambda e: e.tensor_tensor(out=off, in0=incl, in1=pc, op=ALU.subtract), R_, R_)
        baseS = R4[:, 0:512]
        prodS = R4[:, 512:1024]
        cmpS = R4[:, 1024:2560]
        dve(lambda e: e.tensor_tensor(out=baseS.rearrange("p (i n) -> p i n", i=NT), in0=pb[3][:, :].rearrange("p (i n) -> p i n", i=NT),
                                      in1=off.unsqueeze(1).to_broadcast([P, NT, 32]), op=ALU.add), [PB[3]] + R_, [B_r4])
        for k, skb in ((0, s1b), (1, s2b)):
            dve(lambda e, skb=skb: e.tensor_tensor(out=prodS, in0=baseS, in1=skb[:], op=ALU.mult), [B_r4, B_rs], [B_r4])
            dve(lambda e, k=k: e.tensor_reduce(out=slotf[:, k * NT:(k + 1) * NT], in_=prodS.rearrange("p (i n) -> p i n", i=NT), axis=AX.X, op=ALU.add),
                [B_r4], [B_rs])
        dve(lambda e: e.tensor_copy(out=sloti[:], in_=slotf[:]), [B_rs], [B_rs])
        dve(lambda e: e.tensor_tensor(out=cmpS.rearrange("p (t n) -> p t n", t=NIT), in0=incl.unsqueeze(1).to_broadcast([P, NIT, 32]),
                                      in1=thr[:].unsqueeze(2).to_broadcast([P, NIT, 32]), op=ALU.is_le), R_ + [cB["thr"]], [B_r4])
        dve(lambda e: e.tensor_reduce(out=eitf[:], in_=cmpS.rearrange("p (t n) -> p t n", t=NIT), axis=AX.X, op=ALU.add), [B_r4], [B_rs])
        dve(lambda e: e.tensor_scalar(out=eitf[:], in0=eitf[:], scalar1=256.0, scalar2=p2[:, 0:1], op0=ALU.mult, op1=ALU.add),
            [B_rs, cB["p2"]], [B_rs])
        widx3 = widx[:].rearrange("p (t a) -> p t a", a=2)
        dve(lambda e: e.tensor_copy(out=widx3[:, :, 0], in_=eitf[:]), [B_rs], [B_rs])
        dve(lambda e: e.tensor_scalar(out=eitf[:], in0=eitf[:], scalar1=1.0, scalar2=None, op0=ALU.add), [B_rs], [B_rs])
        dve(lambda e: e.tensor_copy(out=widx3[:, :, 1], in_=eitf[:]), [B_rs], [B_rs])

        scat_toks = []
        for i in range(NT):
            for k in range(2):
                scat_toks.append(S.idma(out=xs[:, :], out_off=sloti[:, k * NT + i:k * NT + i + 1], in_=hn2b[:, i, :], in_off=None,
                                        sbuf_buf=B_hn2b[i], reads=[B_hn2b[i], B_rs], writes=[], bounds_check=NSLOT - 1))

        if stage == "sp_dbg":
            dbg = R2[:, 8192:9216]
            B_dbg = Buf("dbg", after=B_hn2f2 + B_hn2Tf2)
            dve(lambda e: e.memset(dbg, 0.0), [], [B_dbg])
            for (c0, n, src, rd) in ((0, 32, slotf[:], [B_rs]), (32, 32, sloti[:], [B_rs]), (64, 48, eitf[:], [B_rs]), (112, 96, widx[:], [B_rs]),
                                     (208, 32, pc, [B_rt]), (240, 32, incl, [B_rt]), (272, 32, off, [B_rt]), (304, 32, pb[2][:, 0:32], [PB[2]]),
                                     (336, 32, wts[:], [B_rs]), (368, 512, pb[3][:, :], [PB[3]])):
                dve(lambda e, c0=c0, n=n, src=src: e.tensor_copy(out=dbg[:, c0:c0 + n], in_=src), rd, [B_dbg])
            t = S.dma("sp", y_t[0], dbg, B_dbg, reads=[B_dbg])
            S.wait_tok("sp", t)
            for t in scat_toks:
                S.wait_tok("sp", t)
            return nc
        if stage == "sp_scatter":
            for t in scat_toks:
                S.wait_tok("sp", t)
            finish(lambda i: h1[:, i, :], B_h1)
            return nc
        old_R3 = allq + [b for row in B_k for b in row] + B_v + B_msb
        wg_sb, wu_sb, wd_sb, B_wg, B_wu, B_wd, wflat = [], [], [], [], [], [], []
        NSL = 2
        for sl in range(NSL):
            o = sl * 6144
            RR = R3 if sl < 2 else R2
            o = o if sl < 2 else 0
            fl = [bf(RR[:, o + m * 2048:o + (m + 1) * 2048]) for m in range(3)]
            wflat.append(fl)
            wg_sb.append(fl[0].rearrange("p (c n) -> p c n", c=8))
            wu_sb.append(fl[1].rearrange("p (c n) -> p c n", c=8))
            wd_sb.append(fl[2].rearrange("p (c n) -> p c n", c=4))
            aft = old_R3 if sl < 2 else B_hn2b
            B_wg.append(Buf("wg%d" % sl, after=aft))
            B_wu.append(Buf("wu%d" % sl, after=aft))
            B_wd.append(Buf("wd%d" % sl, after=aft))
        xin = [bf(R4[:, k * 1024:(k + 1) * 1024]).rearrange("p (a d) -> p a d", a=2) for k in range(2)]
        B_xin = [Buf("xin%d" % k, after=[B_r4]) for k in range(2)]
        xT = bf(R4[:, 2048:3072]).rearrange("p (c n) -> p c n", c=8)
        B_xT = Buf("xT", after=[B_r4])
        actT = bf(R4[:, 3072:3584]).rearrange("p (c n) -> p c n", c=4)
        B_actT = [Buf("actT%d" % c, after=[B_r4]) for c in range(4)]
        sgT = [R4[:, 3584 + k * 256:3840 + k * 256] for k in range(2)]
        B_sgT = [Buf("sgT%d" % k, after=[B_r4]) for k in range(2)]
        ysb = [R2[:, 8192 + k * 2048:10240 + k * 2048].rearrange("p (a d) -> p a d", a=2) for k in range(2)]
        B_ysb = [Buf("ysb%d" % k, after=B_hn2f2 + B_hn2Tf2) for k in range(2)]
        PBh = [[Buf("pb%d_h%d" % (b, h), after=[PB[b]]) for h in range(2)] for b in (2, 3)]
        xs_t = xs.rearrange("(t a p) d -> t p a d", a=2, p=P)
        ys_t = ys.rearrange("(t a p) d -> t p a d", a=2, p=P)

        NSTG = 4
        stg = [R2[:, k * 2048:(k + 1) * 2048] for k in range(NSTG)]
        B_stg = [Buf("stg%d" % k, after=B_hn2b) for k in range(NSTG)]
        pieces = [(it_, m_, a_) for it_ in range(NRUN) for m_ in range(3) for a_ in range(2)]
        wdrams = (w_gate, w_up, w_down)

        def gather_piece(g):
            if g >= len(pieces):
                return
            it_, m_, a_ = pieces[g]
            sg_ = g % NSTG
            S.idma(out=stg[sg_], out_off=None, in_=wdrams[m_][:, :], in_off=widx[:, it_ * 2 + a_:it_ * 2 + a_ + 1],
                   sbuf_buf=B_stg[sg_], reads=[B_rs], writes=[B_stg[sg_]], bounds_check=8191)

        def cast_piece(it, j):
            if it >= NRUN:
                return
            g = it * 6 + j
            it_, m, a = pieces[g]
            sg_ = g % NSTG
            sl = it % NSL
            Bw = (B_wg, B_wu, B_wd)[m][sl]
            dst = wflat[sl][m][:, a * 2048:(a + 1) * 2048]
            if j % 2 == 0:
                S.op("act", lambda e, dst=dst, sg_=sg_: e.activation(out=dst, in_=stg[sg_], func=AF.Copy), reads=[B_stg[sg_]], writes=[Bw])
            else:
                S.op("dve", lambda e, dst=dst, sg_=sg_: e.tensor_copy(out=dst, in_=stg[sg_]), reads=[B_stg[sg_]], writes=[Bw])
            gather_piece(g + NSTG)

        def load_w(it):
            if "n" in DBG:
                RR = R3 if it % 2 == 0 else R2
                Bx = B_nocast[it % 2]
                for m, wdram in enumerate((w_gate, w_up, w_down)):
                    for a in range(2):
                        o = (m * 2 + a) * 2048
                        S.idma(out=RR[:, o:o + 2048], out_off=None, in_=wdram[:, :], in_off=widx[:, it * 2 + a:it * 2 + a + 1],
                               sbuf_buf=Bx[m * 2 + a], reads=[B_rs], writes=[Bx[m * 2 + a]], bounds_check=8191)
                return
            sl = it % NSL
            for m, (wdram, Bw) in enumerate(((w_gate, B_wg[sl]), (w_up, B_wu[sl]), (w_down, B_wd[sl]))):
                for a in range(2):
                    S.idma(out=wflat[sl][m][:, a * 2048:(a + 1) * 2048], out_off=None, in_=wdram[:, :], in_off=widx[:, it * 2 + a:it * 2 + a + 1],
                           sbuf_buf=Bw, reads=[B_rs], writes=[Bw], bounds_check=8191)

        def load_x(it):
            k = it % 2
            S.dma("sp", xin[k], xs_t[it], B_xin[k], writes=[B_xin[k]])

        def TR(it):
            k = it % 2
            for a in range(2):
                tpa = bf(pb[a][:])
                S.ops("pe", [lambda e, c=c, a=a, k=k, tpa=tpa: e.transpose(out=tpa[:, c * 128:(c + 1) * 128], in_=xin[k][:, a, c * 128:(c + 1) * 128],
                                                                          identity=ident[:]) for c in range(8)],
                      reads=[B_xin[k], cB["ident"]], writes=[PB[a]])

        def TR_evac(it):
            for a in range(2):
                tpa = bf(pb[a][:]).rearrange("p (c n) -> p c n", c=8)
                if a == 0:
                    S.op("act", lambda e, tpa=tpa, a=a: e.activation(out=xT[:, :, a * 128:(a + 1) * 128], in_=tpa, func=AF.Copy),
                         reads=[PB[a]], writes=[B_xT])
                else:
                    S.op("dve", lambda e, tpa=tpa, a=a: e.tensor_copy(out=xT[:, :, a * 128:(a + 1) * 128], in_=tpa),
                         reads=[PB[a]], writes=[B_xT])

        def GU(it):
            sl = it % NSL
            for fc in range(4):
                h = fc % 2
                bk = 2 + h
                S.ops("pe", [lambda e, c=c, sl=sl, fc=fc, bk=bk: e.matmul(pb[bk][:, 0:256], lhsT=wg_sb[sl][:, c, fc * 128:(fc + 1) * 128],
                                                                         rhs=xT[:, c, :], start=(c == 0), stop=(c == 7)) for c in range(8)] +
                            [lambda e, c=c, sl=sl, fc=fc, bk=bk: e.matmul(pb[bk][:, 256:512], lhsT=wu_sb[sl][:, c, fc * 128:(fc + 1) * 128],
                                                                         rhs=xT[:, c, :], start=(c == 0), stop=(c == 7)) for c in range(8)],
                      reads=[B_wg[sl], B_wu[sl], B_xT], writes=[PB[bk]])
                S.op("act", lambda e, h=h, bk=bk: e.activation(out=sgT[h], in_=pb[bk][:, 0:256], func=AF.Silu),
                     reads=[PB[bk]], writes=[B_sgT[h]])
                S.op("dve", lambda e, h=h, fc=fc, bk=bk: e.tensor_tensor(out=actT[:, fc, :], in0=pb[bk][:, 256:512], in1=sgT[h], op=ALU.mult),
                     reads=[PB[bk], B_sgT[h]], writes=[B_actT[fc]])
                cast_piece(it + 1, fc)

        y_toks = []
        B_nocast = [[Buf("nc%d_%d" % (k, m), after=old_R3 + B_hn2b + B_hn2f2 + B_hn2Tf2) for m in range(6)] for k in range(2)]

        def DOWN(it):
            sl, k = it % NSL, it % 2
            for a in range(2):
                for half in range(2):
                    by = 4 + a * 2 + half
                    S.ops("pe", [lambda e, fc=fc, sl=sl, a=a, half=half, by=by: e.matmul(
                        pb[by][:, :], lhsT=actT[:, fc, a * 128:(a + 1) * 128], rhs=wd_sb[sl][:, fc, half * 512:(half + 1) * 512],
                        start=(fc == 0), stop=(fc == 3)) for fc in range(4)], reads=B_actT + [B_wd[sl]], writes=[PB[by]])
                    if half == 0:
                        S.op("act", lambda e, a=a, half=half, by=by, k=k: e.activation(out=ysb[k][:, a, half * 512:(half + 1) * 512], in_=pb[by][:, :],
                                                                                     func=AF.Copy), reads=[PB[by]], writes=[B_ysb[k]])
                    else:
                        S.op("dve", lambda e, a=a, half=half, by=by, k=k: e.tensor_copy(out=ysb[k][:, a, half * 512:(half + 1) * 512], in_=pb[by][:, :]),
                             reads=[PB[by]], writes=[B_ysb[k]])
                cast_piece(it + 1, 4 + a)
            if "y" in DBG:
                y_toks.append(S.dma("sp", ys_t[it], ysb[k], B_ysb[k], reads=[B_ysb[k]]))

        for g in range(NSTG):
            gather_piece(g)
        for j in range(6):
            cast_piece(0, j)
        for t in scat_toks:
            S.wait_tok("sp", t)
        load_x(0)
        load_x(1)
        TR(0)
        TR_evac(0)
        for it in range(NRUN):
            GU(it)
            if it + 1 < NRUN:
                TR(it + 1)
            DOWN(it)
            if it + 1 < NRUN:
                TR_evac(it + 1)
            if it + 2 < NRUN:
                load_x(it + 2)
        if stage == "sp_moe":
            for t in y_toks:
                S.wait_tok("sp", t)
            finish(lambda i: h1[:, i, :], B_h1)
            return nc
        NYG = 6
        yg = [[R3[:, (k * 2 + j) * 1024:(k * 2 + j + 1) * 1024] for j in range(2)] for k in range(NYG)]
        B_yg = [[Buf("yg%d_%d" % (k, j), after=B_wg + B_wu + B_wd) for j in range(2)] for k in range(NYG)]
        oring = [R2[:, k * 1024:(k + 1) * 1024] for k in range(2)]
        B_or = [Buf("or%d" % k, after=B_hn2b + B_stg) for k in range(2)]
        for t in y_toks:
            S.wait_tok("pool", t)
        out_toks = []
        for i in range(NT):
            k = i % NYG
            for j in range(2):
                S.idma(out=yg[k][j], out_off=None, in_=ys[:, :], in_off=sloti[:, j * NT + i:j * NT + i + 1], sbuf_buf=B_yg[k][j],
                       reads=[B_rs], writes=[B_yg[k][j]], bounds_check=NSLOT - 1)
            for j in range(2):
                S.op("dve", lambda e, i=i, j=j, k=k: e.scalar_tensor_tensor(out=h1[:, i, :], in0=yg[k][j], scalar=wts3[:, i, j:j + 1], in1=h1[:, i, :],
                                                                            op0=ALU.mult, op1=ALU.add),
                     reads=[B_yg[k][j], B_rs, B_h1[i]], writes=[B_h1[i]])
            ko = i % 2
            c8 = 16 + 2 * i
            S.op("act", lambda e, i=i, ko=ko, c8=c8: e.activation(out=oring[ko], in_=h1[:, i, :], func=AF.Square, accum_out=stat[:, c8:c8 + 1]),
                 reads=[B_h1[i]], writes=[B_or[ko], B_st6[i]])
            rstd_act(c8, D, B_st6[i])
            S.op("dve", lambda e, i=i, ko=ko, c8=c8: e.scalar_tensor_tensor(out=oring[ko], in0=h1[:, i, :], scalar=stat[:, c8 + 1:c8 + 2], in1=gb_fin[:],
                                                                            op0=ALU.mult, op1=ALU.mult),
                 reads=[B_h1[i], B_st6[i], cB["gb_fin"]], writes=[B_or[ko]])
            out_toks.append(S.dma("sp", y_t[i], oring[ko], B_or[ko], reads=[B_or[ko]]))
        for t in out_toks:
            S.wait_tok("sp", t)
    return nc


def _prep_inputs(inputs):
    f = lambda a: np.ascontiguousarray(np.asarray(a, dtype=np.float32))
    w_r = np.concatenate([f(inputs["w_router_group"])[0],
                          np.transpose(f(inputs["w_router_expert"])[0], (1, 0, 2)).reshape(D, 32)], axis=1)
    b_r = np.concatenate([f(inputs["b_router_group"])[0], f(inputs["b_router_expert"])[0].reshape(32)], axis=0)
    shared = {
        "w_in": f(inputs["w_in"])[0],
        "w_out": f(inputs["w_out"])[0],
        "w_gate": np.ascontiguousarray(f(inputs["w_gate"])[0].reshape(32, 8, 128, 512).transpose(0, 2, 1, 3)).reshape(8192, 2048),
        "w_up": np.ascontiguousarray(f(inputs["w_up"])[0].reshape(32, 8, 128, 512).transpose(0, 2, 1, 3)).reshape(8192, 2048),
        "w_down": np.ascontiguousarray(f(inputs["w_down"])[0].reshape(32, 4, 128, 1024).transpose(0, 2, 1, 3)).reshape(8192, 2048),
        "w_spatial": f(inputs["w_spatial"])[0],
        "b_spatial": f(inputs["b_spatial"])[0],
        "attn_norm_g": f(inputs["attn_norm_g"])[0],
        "ffn_norm_g": f(inputs["ffn_norm_g"])[0],
        "final_norm_g": f(inputs["final_norm_g"]),
        "sg_norm_g": f(inputs["sg_norm_g"])[0],
        "sb_out_norm_g": f(inputs["sb_out_norm_g"])[0],
        "sg_out_norm_g": f(inputs["sg_out_norm_g"])[0],
        "w_router": np.ascontiguousarray(w_r),
        "b_router": np.ascontiguousarray(b_r),
    }
    xs = f(inputs["x"])
    return [dict(shared, x=np.ascontiguousarray(xs[b])) for b in range(8)]


def kernel(_stage="full", **inputs):
    in_maps = _prep_inputs(inputs)
    nc = build_nc(_stage)
    res = run_bass_kernel_spmd(nc, in_maps, core_ids=list(range(8)))
    return np.stack([np.asarray(r["y"], dtype=np.float32) for r in res.results], axis=0)
```

```python
import numpy as np
from contextlib import ExitStack
import concourse.bass as bass
import concourse.mybir as mybir
from concourse.bass_utils import run_bass_kernel_spmd

F32 = mybir.dt.float32
BF16 = mybir.dt.bfloat16
I32 = mybir.dt.int32
AF = mybir.ActivationFunctionType
ALU = mybir.AluOpType
AX = mybir.AxisListType

P = 128
T = 2048
NT = 16
D = 1024
KC = 8
EPS = 1e-6


class Buf:
    def __init__(self, name, after=()):
        self.name = name
        self.w = None
        self.r = {}
        self.dsem = None
        self.dcnt = 0
        for b in after:
            if b.w is not None:
                self.add_read(b.w)
            for t in b.r.values():
                self.add_read(t)

    def add_read(self, tok):
        key = id(tok[0])
        old = self.r.get(key)
        if old is None or old[1] < tok[1]:
            self.r[key] = tok


class Sched:
    COMPUTE = ("pe", "act", "dve", "pool")

    def __init__(self, nc, es, same_engine_sync=True):
        self.nc = nc
        self.es = es
        self.E = {"pe": nc.tensor, "act": nc.scalar, "dve": nc.vector, "pool": nc.gpsimd, "sp": nc.sync}
        self.csem = {k: es.enter_context(nc.semaphore("cs_" + k)) for k in self.COMPUTE}
        self.ccnt = {k: 0 for k in self.COMPUTE}
        self.waited = {k: {} for k in self.E}
        self.same = same_engine_sync
        self.nsem = 4
        self.all_dma_toks = []

    def _wait(self, eng, tok):
        sem, val, prod = tok
        if prod == eng and (eng == "pe" or not self.same):
            return
        key = id(sem)
        if self.waited[eng].get(key, 0) >= val:
            return
        if prod == eng and prod in self.COMPUTE:
            pass
        self.E[eng].wait_ge(sem, val)
        self.waited[eng][key] = val

    def _deps(self, eng, reads, writes):
        for b in reads:
            if b.w is not None:
                self._wait(eng, b.w)
        for b in writes:
            if b.w is not None:
                self._wait(eng, b.w)
            for t in b.r.values():
                self._wait(eng, t)

    def op(self, eng, fn, reads=(), writes=()):
        self._deps(eng, reads, writes)
        inst = fn(self.E[eng])
        self.ccnt[eng] += 1
        inst.then_inc(self.csem[eng], 1)
        tok = (self.csem[eng], self.ccnt[eng], eng)
        for b in reads:
            b.add_read(tok)
        for b in writes:
            b.w = tok
            b.r = {}
        return tok

    def ops(self, eng, fns, reads=(), writes=()):
        self._deps(eng, reads, writes)
        inst = None
        for fn in fns:
            inst = fn(self.E[eng])
        self.ccnt[eng] += 1
        inst.then_inc(self.csem[eng], 1)
        tok = (self.csem[eng], self.ccnt[eng], eng)
        for b in reads:
            b.add_read(tok)
        for b in writes:
            b.w = tok
            b.r = {}
        return tok

    def _bufsem(self, b):
        if b.dsem is None:
            b.dsem = self.es.enter_context(self.nc.semaphore("ds_%d" % self.nsem))
            self.nsem += 1
        return b.dsem

    def dma(self, q, out, in_, sbuf_buf, reads=(), writes=(), **kw):
        self._deps(q, reads, writes)
        sem = self._bufsem(sbuf_buf)
        inst = self.E[q].dma_start(out=out, in_=in_, **kw)
        sbuf_buf.dcnt += 16
        inst.then_inc(sem, 16)
        tok = (sem, sbuf_buf.dcnt, "dma")
        for b in reads:
            b.add_read(tok)
        for b in writes:
            b.w = tok
            b.r = {}
        self.all_dma_toks.append(tok)
        return tok

    def idma(self, out, out_off, in_, in_off, sbuf_buf, reads=(), writes=(), bounds_check=None):
        self._deps("pool", reads, writes)
        sem = self._bufsem(sbuf_buf)
        if not hasattr(self, "_bregs"):
            self._bregs = {}
        if bounds_check not in self._bregs:
            self._bregs[bounds_check] = self.nc.gpsimd.to_reg(bounds_check)
        bounds_check = self._bregs[bounds_check]
        inst = self.nc.gpsimd.indirect_dma_start(
            out=out, out_offset=(bass.IndirectOffsetOnAxis(ap=out_off, axis=0) if out_off is not None else None),
            in_=in_, in_offset=(bass.IndirectOffsetOnAxis(ap=in_off, axis=0) if in_off is not None else None),
            bounds_check=bounds_check, oob_is_err=False)
        sbuf_buf.dcnt += 16
        inst.then_inc(sem, 16)
        tok = (sem, sbuf_buf.dcnt, "dma")
        for b in reads:
            b.add_read(tok)
        for b in writes:
            b.w = tok
            b.r = {}
        self.all_dma_toks.append(tok)
        return tok

    def wait_tok(self, eng, tok):
        self._wait(eng, tok)


def _rd_update(b, tok):
    b.r.append(tok)


def build_nc(stage="full", n_experts=32):
    DBG = stage.split(":")[1] if ":" in stage else "wxcy"
    stage = stage.split(":")[0]
    nc = bass.Bass("TRN2", target_bir_lowering=False)
    dt_in = lambda name, shape: nc.dram_tensor(name, shape, F32, kind="ExternalInput").ap()
    x = dt_in("x", [T, D])
    w_in = dt_in("w_in", [D, 2560])
    w_out = dt_in("w_out", [D, D])
    w_gate = dt_in("w_gate", [8192, 2048])
    w_up = dt_in("w_up", [8192, 2048])
    w_down = dt_in("w_down", [8192, 2048])
    w_sp = dt_in("w_spatial", [8, 128, 128])
    b_sp = dt_in("b_spatial", [8, 128])
    g_attn = dt_in("attn_norm_g", [D])
    g_ffn = dt_in("ffn_norm_g", [D])
    g_fin = dt_in("final_norm_g", [D])
    g_sg = dt_in("sg_norm_g", [512])
    g_sbo = dt_in("sb_out_norm_g", [512])
    g_sgo = dt_in("sg_out_norm_g", [512])
    w_r = dt_in("w_router", [D, 36])
    b_r = dt_in("b_router", [36])
    y = nc.dram_tensor("y", [T, D], F32, kind="ExternalOutput").ap()
    xs = nc.dram_tensor("xs_scratch", [48 * 256, D], BF16, kind="Internal").ap()
    ys = nc.dram_tensor("ys_scratch", [48 * 256, D], F32, kind="Internal").ap()

    with ExitStack() as es:
        S = Sched(nc, es)
        sbt = lambda name, shape, dt: es.enter_context(nc.sbuf_tensor(name, shape, dt))
        R1 = sbt("R1", [P, 16384], F32)
        R2 = sbt("R2", [P, 12288], F32)
        R3 = sbt("R3", [P, 12288], F32)
        R4 = sbt("R4", [P, 4096], F32)
        ident = sbt("ident", [P, 128], BF16)
        identf = sbt("identf", [P, 128], F32)
        negU = sbt("negU", [P, 128], BF16)
        negones = sbt("negones", [P, 128], BF16)
        ones = sbt("ones", [P, 128], BF16)
        zeros = sbt("zeros", [P, 512], BF16)
        gc_attn = sbt("gc_attn", [P, 8], F32)
        gc_ffn = sbt("gc_ffn", [P, 8], F32)
        gc_sbo = sbt("gc_sbo", [P, 4], F32)
        gc_sgo = sbt("gc_sgo", [P, 4], F32)
        gb_sg = sbt("gb_sg", [P, 512], F32)
        gb_fin = sbt("gb_fin", [P, D], F32)
        brb = sbt("brb", [P, 36], F32)
        bT = sbt("bT", [P, 8], F32)
        WmT = sbt("WmT", [P, 8 * 128], BF16)
        wr_f = sbt("wr_f", [P, 8 * 36], F32)
        neghalf = sbt("neghalf", [P, 1], F32)
        stat = sbt("stat", [P, 128], F32)
        gu2 = sbt("gu2", [P, 1024], F32)
        gb_ffn = sbt("gb_ffn", [P, D], F32)
        Ustrict = sbt("Ustrict", [P, 128], BF16)
        thr = sbt("thr", [P, 48], F32)
        p2 = sbt("p2", [P, 1], F32)
        s1b = sbt("s1b", [P, NT * 32], BF16)
        s2b = sbt("s2b", [P, NT * 32], BF16)
        selb = sbt("selb", [P, NT * 32], BF16)
        wts = sbt("wts", [P, NT * 2], F32)
        slotf = sbt("slotf", [P, 2 * NT], F32)
        sloti = sbt("sloti", [P, 2 * NT], I32)
        eitf = sbt("eitf", [P, 48], F32)
        widx = sbt("widx", [P, 96], I32)
        rt = sbt("rt", [P, 576], F32)
        rt2 = sbt("rt2", [P, 416], F32)
        lgall = sbt("lgall", [P, NT * 36], F32)
        pb = [es.enter_context(nc.psum_tensor("pb%d" % i, [P, 512], F32)) for i in range(8)]
        PB = [Buf("pb%d" % i) for i in range(8)]

        cB = {n: Buf(n) for n in ["ident", "identf", "negU", "negones", "ones", "zeros", "gc_attn", "gc_ffn",
                                  "gc_sbo", "gc_sgo", "gb_sg", "gb_fin", "brb", "bT", "WmT", "wr_f", "neghalf", "gb_ffn", "Ustrict", "thr", "p2"]}

        def bf(ap):
            return ap.bitcast(BF16)

        S.op("pool", lambda e: e.memset(ident[:], 1.0), writes=[cB["ident"]])
        S.op("pool", lambda e: e.affine_select(out=ident[:], in_=ident[:], pattern=[[-1, 128]], compare_op=ALU.is_equal,
                                               fill=0.0, base=0, channel_multiplier=1), reads=[cB["ident"]], writes=[cB["ident"]])
        S.op("pool", lambda e: e.memset(negU[:], -1.0), writes=[cB["negU"]])
        S.op("pool", lambda e: e.affine_select(out=negU[:], in_=negU[:], pattern=[[-1, 128]], compare_op=ALU.is_ge,
                                               fill=0.0, base=0, channel_multiplier=1), reads=[cB["negU"]], writes=[cB["negU"]])
        S.op("pool", lambda e: e.memset(negones[:], -1.0), writes=[cB["negones"]])
        S.op("pool", lambda e: e.memset(ones[:], 1.0), writes=[cB["ones"]])
        S.op("pool", lambda e: e.memset(zeros[:], 0.0), writes=[cB["zeros"]])
        S.op("pool", lambda e: e.memset(neghalf[:], -0.5), writes=[cB["neghalf"]])
        S.op("pool", lambda e: e.memset(Ustrict[:], 1.0), writes=[cB["Ustrict"]])
        S.op("pool", lambda e: e.affine_select(out=Ustrict[:], in_=Ustrict[:], pattern=[[1, 128]], compare_op=ALU.is_gt,
                                               fill=0.0, base=0, channel_multiplier=-1), reads=[cB["Ustrict"]], writes=[cB["Ustrict"]])
        S.op("pool", lambda e: e.iota(thr[:], pattern=[[256, 48]], base=0, channel_multiplier=0, allow_small_or_imprecise_dtypes=True),
             writes=[cB["thr"]])
        S.op("pool", lambda e: e.iota(p2[:], pattern=[[0, 1]], base=0, channel_multiplier=2, allow_small_or_imprecise_dtypes=True),
             writes=[cB["p2"]])
        S.op("dve", lambda e: e.tensor_copy(out=identf[:], in_=ident[:]), reads=[cB["ident"]], writes=[cB["identf"]])
        with nc.allow_non_contiguous_dma(reason="tiny parameter vectors"):
            S.dma("sp", gc_attn[:], g_attn.rearrange("(c p) -> p c", p=P), cB["gc_attn"], writes=[cB["gc_attn"]])
            S.dma("sp", gc_ffn[:], g_ffn.rearrange("(c p) -> p c", p=P), cB["gc_ffn"], writes=[cB["gc_ffn"]])
            S.dma("sp", gc_sbo[:], g_sbo.rearrange("(c p) -> p c", p=P), cB["gc_sbo"], writes=[cB["gc_sbo"]])
            S.dma("sp", gc_sgo[:], g_sgo.rearrange("(c p) -> p c", p=P), cB["gc_sgo"], writes=[cB["gc_sgo"]])
            S.dma("sp", bT[:], b_sp.rearrange("h t -> t h"), cB["bT"], writes=[cB["bT"]])
        S.dma("sp", gb_sg[:], g_sg.partition_broadcast(P), cB["gb_sg"], writes=[cB["gb_sg"]])
        S.dma("sp", gb_fin[:], g_fin.partition_broadcast(P), cB["gb_fin"], writes=[cB["gb_fin"]])
        S.dma("sp", gb_ffn[:], g_ffn.partition_broadcast(P), cB["gb_ffn"], writes=[cB["gb_ffn"]])
        S.dma("sp", brb[:], b_r.partition_broadcast(P), cB["brb"], writes=[cB["brb"]])
        S.dma("sp", wr_f[:].rearrange("p (c n) -> p c n", c=8), w_r.rearrange("(c p) n -> p c n", p=P), cB["wr_f"], writes=[cB["wr_f"]])

        wsp_f = R4[:, 0:1024].rearrange("p (h s) -> p h s", h=8)
        wsp_b = bf(R4[:, 1024:1536])
        B_wspf = Buf("wspf")
        B_wspb = Buf("wspb")
        S.dma("sp", wsp_f, w_sp.rearrange("h t s -> t h s"), B_wspf, writes=[B_wspf])
        S.op("pool", lambda e: e.affine_select(out=wsp_b.rearrange("p (h s) -> p h s", h=8), in_=wsp_f,
                                               pattern=[[0, 8], [-1, 128]], compare_op=ALU.is_ge, fill=0.0, base=0,
                                               channel_multiplier=1), reads=[B_wspf], writes=[B_wspb])
        tpv = bf(pb[0][:])
        for h in range(8):
            S.op("pe", lambda e, h=h: e.transpose(out=tpv[:, h * 128:(h + 1) * 128], in_=wsp_b[:, h * 128:(h + 1) * 128],
                                                  identity=ident[:]), reads=[B_wspb, cB["ident"]], writes=[PB[0]])
        S.op("dve", lambda e: e.tensor_copy(out=WmT[:], in_=tpv), reads=[PB[0]], writes=[cB["WmT"]])

        win_sb = bf(R1[:, 0:10240]).rearrange("p (c n) -> p c n", c=8)
        B_win = [Buf("win%d" % i) for i in range(5)]
        w_in_v = w_in.rearrange("(c p) n -> p c n", p=P)
        for part in range(5):
            S.dma("pool", win_sb[:, :, part * 512:(part + 1) * 512], w_in_v[:, :, part * 512:(part + 1) * 512],
                  B_win[part], writes=[B_win[part]])

        hnT = bf(R2[:, 0:8192]).rearrange("p (c n) -> p c n", c=8)
        B_hnT = [Buf("hnT%d" % i) for i in range(NT)]
        xring = [R1[:, 10240 + k * 1024:10240 + (k + 1) * 1024] for k in range(2)]
        B_xr = [Buf("xr%d" % k) for k in range(2)]
        xnring = [bf(R1[:, 12288 + k * 512:12288 + (k + 1) * 512]) for k in range(2)]
        B_xn = [Buf("xn%d" % k) for k in range(2)]
        junk = bf(R1[:, 15872:16384])
        B_junk = Buf("junk")
        B_stat = Buf("stat")
        B_st1 = [Buf("st1_%d" % i) for i in range(NT)]
        x_t = x.rearrange("(i p) d -> i p d", p=P)
        y_t = y.rearrange("(i p) d -> i p d", p=P)

        def rstd_from_ss(col, n, buf=None):
            buf = B_stat if buf is None else buf
            S.op("dve", lambda e: e.tensor_scalar(out=stat[:, col + 1:col + 2], in0=stat[:, col:col + 1], scalar1=1.0 / n,
                                                  scalar2=EPS, op0=ALU.mult, op1=ALU.add), reads=[buf], writes=[buf])
            S.op("pool", lambda e: e.tensor_tensor(out=stat[:, col + 1:col + 2], in0=stat[:, col + 1:col + 2], in1=neghalf[:],
                                                   op=ALU.pow), reads=[buf, cB["neghalf"]], writes=[buf])

        def rstd_act(col, n, buf):
            S.op("dve", lambda e: e.tensor_scalar(out=stat[:, col + 1:col + 2], in0=stat[:, col:col + 1], scalar1=1.0 / n,
                                                  scalar2=EPS, op0=ALU.mult, op1=ALU.add), reads=[buf], writes=[buf])
            S.op("act", lambda e: e.activation(out=stat[:, col + 1:col + 2], in_=stat[:, col + 1:col + 2], func=AF.Sqrt), reads=[buf], writes=[buf])
            S.op("dve", lambda e: e.reciprocal(out=stat[:, col + 1:col + 2], in_=stat[:, col + 1:col + 2]), reads=[buf], writes=[buf])

        for i in range(NT):
            k = i % 2
            S.dma("sp", xring[k], x_t[i], B_xr[k], writes=[B_xr[k]])
            c1 = 64 + 2 * i
            S.op("act", lambda e, k=k, c1=c1: e.activation(out=xnring[k], in_=xring[k], func=AF.Square, accum_out=stat[:, c1:c1 + 1]),
                 reads=[B_xr[k]], writes=[B_xn[k], B_st1[i]])
            rstd_from_ss(c1, D, B_st1[i])
            S.op("dve", lambda e, k=k, c1=c1: e.tensor_scalar(out=xnring[k], in0=xring[k], scalar1=stat[:, c1 + 1:c1 + 2], scalar2=None,
                                                              op0=ALU.mult), reads=[B_xr[k], B_st1[i]], writes=[B_xn[k]])
            tb = k
            tpk = bf(pb[tb][:])
            S.ops("pe", [lambda e, c=c, k=k, tpk=tpk: e.transpose(out=tpk[:, c * 128:(c + 1) * 128],
                                                                  in_=xnring[k][:, c * 128:(c + 1) * 128], identity=ident[:])
                         for c in range(8)], reads=[B_xn[k], cB["ident"]], writes=[PB[tb]])
            S.op("dve", lambda e, i=i, tpk=tpk: e.tensor_tensor(out=hnT[:, :, i * 128:(i + 1) * 128],
                                                                in0=tpk.rearrange("p (c n) -> p c n", c=8),
                                                                in1=gc_attn[:].unsqueeze(2).to_broadcast([P, 8, 128]), op=ALU.mult),
                 reads=[PB[tb], cB["gc_attn"]], writes=[B_hnT[i]])

        qT = bf(R3[:, 0:4096]).rearrange("p (j n) -> p j n", j=4)
        kT = bf(R3[:, 4096:8192]).rearrange("p (j n) -> p j n", j=4)
        vS = bf(R3[:, 8192:12288]).rearrange("p (i n) -> p i n", i=NT)
        B_q = [[Buf("q%d_%d" % (j, t)) for t in range(4)] for j in range(4)]
        B_k = [[Buf("k%d_%d" % (j, t)) for t in range(4)] for j in range(4)]
        B_v = [Buf("v%d" % i) for i in range(NT)]
        cnt = 0
        for part, dstT, dstB in ((0, qT, B_q), (1, kT, B_k)):
            for j in range(4):
                for tq in range(4):
                    bank = 2 + (cnt % 2)
                    cnt += 1
                    S.ops("pe", [lambda e, c=c, j=j, tq=tq, part=part, bank=bank: e.matmul(
                        pb[bank][:, :], lhsT=win_sb[:, c, part * 512 + j * 128:part * 512 + (j + 1) * 128],
                        rhs=hnT[:, c, tq * 512:(tq + 1) * 512], start=(c == 0), stop=(c == 7)) for c in range(8)],
                        reads=[B_win[part]] + B_hnT[tq * 4:(tq + 1) * 4], writes=[PB[bank]])
                    if part == 0:
                        S.op("act", lambda e, j=j, tq=tq, bank=bank: e.activation(out=qT[:, j, tq * 512:(tq + 1) * 512], in_=pb[bank][:, :],
                                                                                func=AF.Copy, scale=0.125),
                             reads=[PB[bank]], writes=[dstB[j][tq]])
                    else:
                        S.op("dve", lambda e, j=j, tq=tq, bank=bank: e.tensor_copy(out=kT[:, j, tq * 512:(tq + 1) * 512], in_=pb[bank][:, :]),
                             reads=[PB[bank]], writes=[dstB[j][tq]])
        for i in range(NT):
            bank = 4 + (i % 2)
            S.ops("pe", [lambda e, c=c, i=i, bank=bank: e.matmul(pb[bank][:, :], lhsT=hnT[:, c, i * 128:(i + 1) * 128],
                                                                 rhs=win_sb[:, c, 1024:1536], start=(c == 0), stop=(c == 7))
                         for c in range(8)], reads=[B_win[2], B_hnT[i]], writes=[PB[bank]])
            eng = "act" if i % 2 == 0 else "dve"
            if eng == "act":
                S.op("act", lambda e, i=i, bank=bank: e.activation(out=vS[:, i, :], in_=pb[bank][:, :], func=AF.Copy),
                     reads=[PB[bank]], writes=[B_v[i]])
            else:
                S.op("dve", lambda e, i=i, bank=bank: e.tensor_copy(out=vS[:, i, :], in_=pb[bank][:, :]),
                     reads=[PB[bank]], writes=[B_v[i]])

        mixT_sg = bf(R4[:, 0:4096]).rearrange("p (c n) -> p c n", c=4)
        B_msg = [Buf("msg%d" % i, after=[B_wspf, B_wspb]) for i in range(NT)]
        gu_t = [gu2[:, k * 512:(k + 1) * 512] for k in range(2)]
        gv_t = [R1[:, 13312 + k * 1536:13824 + k * 1536] for k in range(2)]
        m1_t = [R1[:, 13824 + k * 1536:14336 + k * 1536] for k in range(2)]
        vs_t = [bf(R1[:, 14336 + k * 1536:14592 + k * 1536]) for k in range(2)]
        sgn_t = [bf(R1[:, 14592 + k * 1536:14848 + k * 1536]) for k in range(2)]
        B_gu2 = [Buf("gu%d" % k) for k in range(2)]
        B_gv2 = [Buf("gv%d" % k) for k in range(2)]
        B_m12 = [Buf("m1_%d" % k) for k in range(2)]
        B_vs2 = [Buf("vs%d" % k) for k in range(2)]
        B_sgn2 = [Buf("sgn%d" % k) for k in range(2)]
        B_st3 = [Buf("st3_%d" % i) for i in range(NT)]
        B_gu, B_gv, B_vgn, B_m1, B_sg, B_sgn = B_gu2[0], B_gv2[0], B_vs2[0], B_m12[0], B_sgn2[0], B_sgn2[1]
        extra_R1 = [B_gv2[1], B_m12[1], B_vs2[1]]

        def sgA(i):
            k = i % 2
            bu, bv = (2, 3) if k == 0 else (5, 6)
            c3 = 4 * i
            for (bank, col0, dst, dB) in ((bu, 1536, gu_t[k], B_gu2[k]), (bv, 2048, gv_t[k], B_gv2[k])):
                S.ops("pe", [lambda e, c=c, i=i, bank=bank, col0=col0: e.matmul(pb[bank][:, :], lhsT=hnT[:, c, i * 128:(i + 1) * 128],
                                                                                rhs=win_sb[:, c, col0:col0 + 512], start=(c == 0), stop=(c == 7))
                             for c in range(8)], reads=[B_win[3], B_win[4], B_hnT[i]], writes=[PB[bank]])
                S.op("act", lambda e, bank=bank, dst=dst: e.activation(out=dst, in_=pb[bank][:, :], func=AF.Gelu_apprx_tanh),
                     reads=[PB[bank]], writes=[dB])
            S.op("act", lambda e, k=k, c3=c3: e.activation(out=vs_t[k], in_=gv_t[k], func=AF.Square, accum_out=stat[:, c3:c3 + 1]),
                 reads=[B_gv2[k]], writes=[B_vs2[k], B_st3[i]])
            rstd_from_ss(c3, 512, B_st3[i])
            S.op("dve", lambda e, k=k, c3=c3: e.tensor_scalar(out=vs_t[k], in0=gv_t[k], scalar1=stat[:, c3 + 1:c3 + 2], scalar2=None, op0=ALU.mult),
                 reads=[B_gv2[k], B_st3[i]], writes=[B_vs2[k]])

        def sgB(i):
            k = i % 2
            bm = 4 if k == 0 else 7
            c3 = 4 * i + 2
            S.ops("pe", [lambda e, h=h, k=k, bm=bm: e.matmul(pb[bm][:, h * 64:(h + 1) * 64], lhsT=WmT[:, h * 128:(h + 1) * 128],
                                                             rhs=vs_t[k][:, h * 64:(h + 1) * 64], start=True, stop=True) for h in range(8)],
                  reads=[B_vs2[k], cB["WmT"]], writes=[PB[bm]])
            S.op("dve", lambda e, k=k, bm=bm: e.tensor_tensor(out=m1_t[k], in0=pb[bm][:, :], in1=gb_sg[:], op=ALU.mult),
                 reads=[PB[bm], cB["gb_sg"]], writes=[B_m12[k]])
            S.op("dve", lambda e, k=k: e.tensor_tensor(out=m1_t[k].rearrange("p (h d) -> p h d", h=8), in0=m1_t[k].rearrange("p (h d) -> p h d", h=8),
                                                       in1=bT[:].unsqueeze(2).to_broadcast([P, 8, 64]), op=ALU.add),
                 reads=[B_m12[k], cB["bT"]], writes=[B_m12[k]])
            S.op("dve", lambda e, k=k: e.tensor_tensor(out=m1_t[k], in0=m1_t[k], in1=gu_t[k], op=ALU.mult), reads=[B_m12[k], B_gu2[k]], writes=[B_m12[k]])
            S.op("act", lambda e, k=k, c3=c3: e.activation(out=sgn_t[k], in_=m1_t[k], func=AF.Square, accum_out=stat[:, c3:c3 + 1]),
                 reads=[B_m12[k]], writes=[B_sgn2[k], B_st3[i]])
            rstd_from_ss(c3, 512, B_st3[i])
            S.op("dve", lambda e, k=k, c3=c3: e.tensor_scalar(out=sgn_t[k], in0=m1_t[k], scalar1=stat[:, c3 + 1:c3 + 2], scalar2=None, op0=ALU.mult),
                 reads=[B_m12[k], B_st3[i]], writes=[B_sgn2[k]])

        def sgC(i):
            k = i % 2
            tpk = bf(pb[k][:])
            S.ops("pe", [lambda e, c=c, tpk=tpk, k=k: e.transpose(out=tpk[:, c * 128:(c + 1) * 128], in_=sgn_t[k][:, c * 128:(c + 1) * 128],
                                                                  identity=ident[:]) for c in range(4)],
                  reads=[B_sgn2[k], cB["ident"]], writes=[PB[k]])
            S.op("dve", lambda e, i=i, tpk=tpk: e.tensor_tensor(out=mixT_sg[:, :, i * 128:(i + 1) * 128],
                                                                in0=tpk[:, 0:512].rearrange("p (c n) -> p c n", c=4),
                                                                in1=gc_sgo[:].unsqueeze(2).to_broadcast([P, 4, 128]), op=ALU.mult),
                 reads=[PB[k], cB["gc_sgo"]], writes=[B_msg[i]])

        sgA(0)
        sgA(1)
        sgB(0)
        for i in range(NT):
            if i + 2 < NT:
                sgA(i + 2)
            if i + 1 < NT:
                sgB(i + 1)
            sgC(i)

        wout_sb = bf(R2[:, 8192:12288]).rearrange("p (c n) -> p c n", c=8)
        B_wout = Buf("wout")
        S.dma("pool", wout_sb, w_out.rearrange("(c p) n -> p c n", p=P), B_wout, writes=[B_wout])
        old_R1 = B_win + B_xr + B_xn + [B_gu, B_gv, B_vgn, B_m1, B_sg, B_sgn, B_junk] + extra_R1
        oT = R1[:, 0:8192].rearrange("p (j n) -> p j n", j=4)
        B_oT = [[Buf("oT%d_%d" % (j, qc), after=old_R1) for qc in range(4)] for j in range(4)]
        e_t, sp_t, a_t, S_t = [], [], [], []
        B_e, B_sp, B_a, B_S = [], [], [], []
        for h in range(2):
            o = 8192 + h * 2048
            e_t.append(R1[:, o:o + 512])
            sp_t.append([bf(R1[:, o + 512 + k * 256:o + 768 + k * 256]) for k in range(2)])
            a_t.append([bf(R1[:, o + 1024 + k * 256:o + 1280 + k * 256]) for k in range(2)])
            S_t.append([bf(R1[:, o + 1536 + k * 256:o + 1792 + k * 256]) for k in range(2)])
            B_e.append(Buf("e%d" % h, after=old_R1))
            B_sp.append([Buf("sp%d_%d" % (h, k), after=old_R1) for k in range(2)])
            B_a.append([Buf("a%d_%d" % (h, k), after=old_R1) for k in range(2)])
            B_S.append([Buf("S%d_%d" % (h, k), after=old_R1) for k in range(2)])

        units = []
        gi = 0
        for j in range(4):
            for qc in range(4):
                kbs = list(range(4 * qc + 3, -1, -1))
                for ui, kb in enumerate(kbs):
                    r = kb - 4 * qc
                    c0 = 128 * r if r >= 0 else 0
                    units.append(dict(j=j, qc=qc, kb=kb, r=r, c0=c0, N=512 - c0, ui=ui, first=(ui == 0),
                                      last=(ui == len(kbs) - 1), gi=gi))
                gi += 1
        nU = len(units)

        def qk_mm(e, out_ap, U, h, start, stop):
            b = 64 * h
            j, kb, qc, c0, N = U["j"], U["kb"], U["qc"], U["c0"], U["N"]
            q0 = qc * 512 + c0
            return e.matmul(out_ap, lhsT=kT[b:b + 64, j, kb * 128:(kb + 1) * 128], rhs=qT[b:b + 64, j, q0:q0 + N],
                            start=start, stop=stop)

        def stA(u):
            U = units[u]
            for h in range(2):
                zb = 2 * h + (u % 2)
                S.op("pe", lambda e, U=U, h=h, zb=zb: qk_mm(e, pb[zb][:, 0:U["N"]], U, h, True, True),
                     reads=[B_k[U["j"]][U["kb"] // 4], B_q[U["j"]][U["qc"]]], writes=[PB[zb]])

        def stB(u):
            U = units[u]
            N = U["N"]
            for h in range(2):
                zb = 2 * h + (u % 2)
                S.op("act", lambda e, h=h, zb=zb, N=N: e.activation(out=e_t[h][:, 0:N], in_=pb[zb][:, 0:N], func=AF.Exp),
                     reads=[PB[zb]], writes=[B_e[h]])
                S.op("act", lambda e, h=h, N=N, u=u: e.activation(out=sp_t[h][u % 2][:, 0:N], in_=e_t[h][:, 0:N], func=AF.Ln, bias=1.0),
                     reads=[B_e[h]], writes=[B_sp[h][u % 2]])

        def mask_tile(ap, bufs):
            S.op("pool", lambda e: e.affine_select(out=ap, in_=ap, pattern=[[1, 128]], compare_op=ALU.is_gt, fill=0.0, base=0,
                                                   channel_multiplier=-1), reads=bufs, writes=bufs)

        def stC(u):
            U = units[u]
            if U["r"] >= 0:
                for h in range(2):
                    mask_tile(sp_t[h][u % 2][:, 0:128], [B_sp[h][u % 2]])

        def stD(u):
            U = units[u]
            N, c0, ui = U["N"], U["c0"], U["ui"]
            cur, nxt = ui % 2, 1 - (ui % 2)
            for h in range(2):
                if U["first"]:
                    S.op("dve", lambda e, h=h: e.memset(S_t[h][0][:, :], 0.0), writes=[B_S[h][0]])
                    S.op("dve", lambda e, h=h: e.memset(S_t[h][1][:, :], 0.0), writes=[B_S[h][1]])
                S.ops("pe", [
                    lambda e, h=h, N=N, u=u: e.matmul(pb[4 + h][:, 0:N], lhsT=negU[:], rhs=sp_t[h][u % 2][:, 0:N], start=True, stop=False),
                    lambda e, h=h, N=N, c0=c0, cur=cur: e.matmul(pb[4 + h][:, 0:N], lhsT=negones[:], rhs=S_t[h][cur][:, c0:512], start=False, stop=False),
                    lambda e, h=h, U=U: qk_mm(e, pb[4 + h][:, 0:U["N"]], U, h, False, True)],
                    reads=[B_sp[h][u % 2], B_S[h][cur], B_k[U["j"]][U["kb"] // 4], B_q[U["j"]][U["qc"]], cB["negU"], cB["negones"]],
                    writes=[PB[4 + h]])
                if not U["last"]:
                    S.op("dve", lambda e, h=h, N=N, c0=c0, cur=cur, nxt=nxt, u=u: e.tensor_tensor(
                        out=S_t[h][nxt][:, c0:512], in0=S_t[h][cur][:, c0:512], in1=sp_t[h][u % 2][:, 0:N], op=ALU.add),
                        reads=[B_S[h][cur], B_sp[h][u % 2]], writes=[B_S[h][nxt]])

        def stE(u):
            U = units[u]
            N = U["N"]
            for h in range(2):
                S.op("act", lambda e, h=h, N=N, u=u: e.activation(out=a_t[h][u % 2][:, 0:N], in_=pb[4 + h][:, 0:N], func=AF.Exp),
                     reads=[PB[4 + h]], writes=[B_a[h][u % 2]])

        def stF(u):
            U = units[u]
            if U["r"] >= 0:
                for h in range(2):
                    mask_tile(a_t[h][u % 2][:, 0:128], [B_a[h][u % 2]])

        def stG(u):
            U = units[u]
            j, kb, qc, c0, N = U["j"], U["kb"], U["qc"], U["c0"], U["N"]
            ob = 6 + (U["gi"] % 2)
            if U["first"]:
                for h in range(2):
                    S.op("pe", lambda e, ob=ob, h=h: e.matmul(pb[ob][64 * h:64 * h + 64, :], lhsT=zeros[:, 0:64], rhs=zeros[:, :], start=True, stop=False),
                         reads=[cB["zeros"]], writes=[PB[ob]])
            for h in range(2):
                hh = 2 * j + h
                S.op("pe", lambda e, h=h, hh=hh, ob=ob, kb=kb, c0=c0, N=N, u=u, U=U: e.matmul(
                    pb[ob][64 * h:64 * h + 64, c0:512], lhsT=vS[:, kb, hh * 64:(hh + 1) * 64], rhs=a_t[h][u % 2][:, 0:N],
                    start=False, stop=U["last"]), reads=[B_a[h][u % 2], B_v[kb]], writes=[PB[ob]])
            if U["last"]:
                S.op("dve", lambda e, j=j, qc=qc, ob=ob: e.tensor_copy(out=oT[:, j, qc * 512:(qc + 1) * 512], in_=pb[ob][:, :]),
                     reads=[PB[ob]], writes=[B_oT[j][qc]])

        stA(0)
        stA(1)
        stB(0)
        stC(0)
        for u in range(nU):
            if u + 2 < nU:
                stA(u + 2)
            stD(u)
            if u >= 1:
                stG(u - 1)
            if u + 1 < nU:
                stB(u + 1)
                stC(u + 1)
            stE(u)
            stF(u)
        stG(nU - 1)

        allq = [b for row in B_q for b in row]
        mixT_sb = bf(R3[:, 0:4096]).rearrange("p (c n) -> p c n", c=4)
        B_msb = [Buf("msb%d" % t, after=allq) for t in range(4)]
        att_work = B_e + [b for l in B_sp + B_a + B_S for b in l]
        sq_t = [bf(R1[:, 12288 + k * 256:12544 + k * 256]) for k in range(4)]
        B_sq = [Buf("sq%d" % k, after=old_R1) for k in range(4)]
        rb = R1[:, 13312:13824]
        B_rb = Buf("rb", after=old_R1)
        for tq in range(4):
            for j in range(4):
                S.op("act", lambda e, j=j, tq=tq: e.activation(out=sq_t[j][:, :], in_=oT[:, j, tq * 512:(tq + 1) * 512], func=AF.Square),
                     reads=[B_oT[j][tq]], writes=[B_sq[j]])
            S.ops("pe", [lambda e, j=j: e.matmul(pb[0][:, :], lhsT=ones[:], rhs=sq_t[j][:, :], start=(j == 0), stop=(j == 3))
                         for j in range(4)], reads=B_sq + [cB["ones"]], writes=[PB[0]])
            S.op("dve", lambda e: e.tensor_scalar(out=rb, in0=pb[0][:, :], scalar1=1.0 / 512, scalar2=EPS, op0=ALU.mult, op1=ALU.add),
                 reads=[PB[0]], writes=[B_rb])
            S.op("act", lambda e: e.activation(out=rb, in_=rb, func=AF.Sqrt), reads=[B_rb], writes=[B_rb])
            S.op("dve", lambda e: e.reciprocal(out=rb, in_=rb), reads=[B_rb], writes=[B_rb])
            for j in range(4):
                S.op("dve", lambda e, j=j, tq=tq: e.scalar_tensor_tensor(out=mixT_sb[:, j, tq * 512:(tq + 1) * 512],
                                                                         in0=oT[:, j, tq * 512:(tq + 1) * 512], scalar=gc_sbo[:, j:j + 1],
                                                                         in1=rb, op0=ALU.mult, op1=ALU.mult),
                     reads=[B_oT[j][tq], B_rb, cB["gc_sbo"]], writes=[B_msb[tq]])

        h1 = R1[:, :].rearrange("p (i d) -> p i d", i=NT)
        all_R1_p4 = [b for row in B_oT for b in row] + att_work + B_sq + [B_rb]
        B_h1 = [Buf("h1_%d" % i, after=all_R1_p4) for i in range(NT)]
        xr2 = [R2[:, k * 1024:(k + 1) * 1024] for k in range(2)]
        B_xr2 = [Buf("xr2_%d" % k, after=B_hnT) for k in range(2)]
        for i in range(NT):
            k = i % 2
            S.dma("sp", xr2[k], x_t[i], B_xr2[k], writes=[B_xr2[k]])
            for half in range(2):
                bank = (2 * i + half) % 4
                S.ops("pe", [lambda e, c=c, i=i, half=half, bank=bank: e.matmul(
                    pb[bank][:, :], lhsT=(mixT_sb[:, c, i * 128:(i + 1) * 128] if c < 4 else mixT_sg[:, c - 4, i * 128:(i + 1) * 128]),
                    rhs=wout_sb[:, c, half * 512:(half + 1) * 512], start=(c == 0), stop=(c == 7)) for c in range(8)],
                    reads=[B_msb[i // 4], B_msg[i], B_wout], writes=[PB[bank]])
                S.op("dve", lambda e, i=i, half=half, bank=bank, k=k: e.tensor_tensor(
                    out=h1[:, i, half * 512:(half + 1) * 512], in0=pb[bank][:, :], in1=xr2[k][:, half * 512:(half + 1) * 512], op=ALU.add),
                    reads=[PB[bank], B_xr2[k]], writes=[B_h1[i]])

        def finish(src_tiles, src_bufs):
            toks = []
            for i in range(NT):
                toks.append(S.dma("sp", y_t[i], src_tiles(i), src_bufs[i], reads=[src_bufs[i]]))
            for t in toks:
                S.wait_tok("sp", t)

        if stage == "h1":
            finish(lambda i: h1[:, i, :], B_h1)
            return nc

        NIT = 48
        NRUN = 47
        NSLOT = NIT * 256
        hn2b = bf(R2[:, 0:8192]).rearrange("p (i d) -> p i d", i=NT)
        B_hn2b = [Buf("hn2b%d" % i, after=B_xr2 + B_hnT) for i in range(NT)]
        B_rt = Buf("rt")
        B_rs = Buf("routing_state")
        s1b3 = s1b[:].rearrange("p (i n) -> p i n", i=NT)
        s2b3 = s2b[:].rearrange("p (i n) -> p i n", i=NT)
        selb3 = selb[:].rearrange("p (i n) -> p i n", i=NT)
        wts3 = wts[:].rearrange("p (i n) -> p i n", i=NT)

        def dve(fn, reads, writes):
            S.op("dve", fn, reads=reads, writes=writes)

        hn2f2 = [R2[:, 8192 + k * 1024:9216 + k * 1024] for k in range(2)]
        hn2Tf2 = [R2[:, 10240 + k * 1024:11264 + k * 1024].rearrange("p (c n) -> p c n", c=8) for k in range(2)]
        B_hn2f2 = [Buf("hn2f_%d" % k, after=[B_wout]) for k in range(2)]
        B_hn2Tf2 = [Buf("hn2Tf_%d" % k, after=[B_wout]) for k in range(2)]
        B_st6 = [Buf("st6_%d" % i) for i in range(NT)]
        B_lg = [Buf("lg%d" % i) for i in range(NT)]
        lg3 = lgall[:].rearrange("p (i n) -> p i n", i=NT)
        for i in range(NT):
            k = i % 2
            tb0, tb1, rb_ = (0, 1, 2) if k == 0 else (4, 5, 6)
            c0s = 16 + 2 * i
            S.op("act", lambda e, i=i, c0s=c0s: e.activation(out=hn2b[:, i, :], in_=h1[:, i, :], func=AF.Square, accum_out=stat[:, c0s:c0s + 1]),
                 reads=[B_h1[i]], writes=[B_hn2b[i], B_st6[i]])
            rstd_from_ss(c0s, D, B_st6[i])
            dve(lambda e, i=i, k=k, c0s=c0s: e.scalar_tensor_tensor(out=hn2f2[k], in0=h1[:, i, :], scalar=stat[:, c0s + 1:c0s + 2], in1=gb_ffn[:],
                                                                    op0=ALU.mult, op1=ALU.mult),
                [B_h1[i], B_st6[i], cB["gb_ffn"]], [B_hn2f2[k]])
            S.op("act", lambda e, i=i, k=k: e.activation(out=hn2b[:, i, :], in_=hn2f2[k], func=AF.Copy), reads=[B_hn2f2[k]], writes=[B_hn2b[i]])
            for hb, tb in ((0, tb0), (1, tb1)):
                S.ops("pe", [lambda e, c=c, tb=tb, k=k: e.transpose(out=pb[tb][:, (c % 4) * 128:(c % 4 + 1) * 128],
                                                                    in_=hn2f2[k][:, c * 128:(c + 1) * 128], identity=identf[:])
                             for c in range(4 * hb, 4 * hb + 4)], reads=[B_hn2f2[k], cB["identf"]], writes=[PB[tb]])
            S.op("act", lambda e, k=k, tb0=tb0: e.activation(out=hn2Tf2[k][:, 0:4, :], in_=pb[tb0][:, :].rearrange("p (c n) -> p c n", c=4), func=AF.Copy),
                 reads=[PB[tb0]], writes=[B_hn2Tf2[k]])
            dve(lambda e, k=k, tb1=tb1: e.tensor_copy(out=hn2Tf2[k][:, 4:8, :], in_=pb[tb1][:, :].rearrange("p (c n) -> p c n", c=4)),
                [PB[tb1]], [B_hn2Tf2[k]])
            S.ops("pe", [lambda e, c=c, k=k, rb_=rb_: e.matmul(pb[rb_][:, 0:36], lhsT=hn2Tf2[k][:, c, :], rhs=wr_f[:, c * 36:(c + 1) * 36],
                                                               start=(c == 0), stop=(c == 7)) for c in range(8)],
                  reads=[B_hn2Tf2[k], cB["wr_f"]], writes=[PB[rb_]])
            dve(lambda e, i=i, rb_=rb_: e.tensor_tensor(out=lg3[:, i, :], in0=pb[rb_][:, 0:36], in1=brb[:], op=ALU.add),
                [PB[rb_], cB["brb"]], [B_lg[i]])

        def rtv(c0, n):
            return rt[:, c0:c0 + n]
        gl = lg3[:, :, 0:4]
        ell = lg3[:, :, 4:36].rearrange("p i (g k) -> p i g k", g=4)
        gmax, gsum, gw = rtv(0, 16), rtv(16, 16), rtv(32, 16)
        m1c, m2c, dlt, ed, w1, w2 = rtv(48, 16), rtv(64, 16), rtv(80, 16), rtv(96, 16), rtv(112, 16), rtv(128, 16)
        goh = rtv(144, 64).rearrange("p (i g) -> p i g", i=NT)
        gex = rtv(208, 64).rearrange("p (i g) -> p i g", i=NT)
        el = rtv(320, 128).rearrange("p (i k) -> p i k", i=NT)
        oh1 = rtv(448, 128).rearrange("p (i k) -> p i k", i=NT)
        el2 = rt2[:, 0:128].rearrange("p (i k) -> p i k", i=NT)
        oh2 = rt2[:, 128:256].rearrange("p (i k) -> p i k", i=NT)
        tmp4 = R4[:, 0:512].rearrange("p (i g k) -> p i g k", i=NT, g=4)
        B_r4 = Buf("r4tmp", after=B_msg)
        R_ = [B_rt]
        LG = B_lg
        dve(lambda e: e.tensor_reduce(out=gmax, in_=gl, axis=AX.X, op=ALU.max), LG, R_)
        dve(lambda e: e.tensor_tensor(out=goh, in0=gl, in1=gmax.unsqueeze(2).to_broadcast([P, NT, 4]), op=ALU.is_equal), LG + R_, R_)
        dve(lambda e: e.tensor_tensor(out=gex, in0=gl, in1=gmax.unsqueeze(2).to_broadcast([P, NT, 4]), op=ALU.subtract), LG + R_, R_)
        S.op("act", lambda e: e.activation(out=gex, in_=gex, func=AF.Exp), reads=R_, writes=R_)
        dve(lambda e: e.tensor_reduce(out=gsum, in_=gex, axis=AX.X, op=ALU.add), R_, R_)
        dve(lambda e: e.reciprocal(out=gw, in_=gsum), R_, R_)
        dve(lambda e: e.tensor_tensor(out=tmp4, in0=ell, in1=goh.unsqueeze(3).to_broadcast([P, NT, 4, 8]), op=ALU.mult), LG + R_, [B_r4])
        dve(lambda e: e.tensor_reduce(out=el, in_=tmp4.rearrange("p i g k -> p i k g"), axis=AX.X, op=ALU.add), [B_r4], R_)
        dve(lambda e: e.tensor_reduce(out=m1c, in_=el, axis=AX.X, op=ALU.max), R_, R_)
        dve(lambda e: e.tensor_tensor(out=oh1, in0=el, in1=m1c.unsqueeze(2).to_broadcast([P, NT, 8]), op=ALU.is_equal), R_, R_)
        dve(lambda e: e.scalar_tensor_tensor(out=rt2[:, 0:128], in0=rtv(448, 128), scalar=-1e30, in1=rtv(320, 128), op0=ALU.mult, op1=ALU.add), R_, R_)
        dve(lambda e: e.tensor_reduce(out=m2c, in_=el2, axis=AX.X, op=ALU.max), R_, R_)
        dve(lambda e: e.tensor_tensor(out=oh2, in0=el2, in1=m2c.unsqueeze(2).to_broadcast([P, NT, 8]), op=ALU.is_equal), R_, R_)
        dve(lambda e: e.tensor_tensor(out=dlt, in0=m2c, in1=m1c, op=ALU.subtract), R_, R_)
        S.op("act", lambda e: e.activation(out=ed, in_=dlt, func=AF.Exp), reads=R_, writes=R_)
        dve(lambda e: e.tensor_scalar(out=w1, in0=ed, scalar1=1.0, scalar2=None, op0=ALU.add), R_, R_)
        dve(lambda e: e.reciprocal(out=w1, in_=w1), R_, R_)
        dve(lambda e: e.tensor_tensor(out=w2, in0=ed, in1=w1, op=ALU.mult), R_, R_)
        dve(lambda e: e.tensor_tensor(out=wts3[:, :, 0], in0=w1, in1=gw, op=ALU.mult), R_, [B_rs])
        dve(lambda e: e.tensor_tensor(out=wts3[:, :, 1], in0=w2, in1=gw, op=ALU.mult), R_, [B_rs])
        s1b4 = s1b[:].rearrange("p (i g k) -> p i g k", i=NT, g=4)
        s2b4 = s2b[:].rearrange("p (i g k) -> p i g k", i=NT, g=4)
        dve(lambda e: e.tensor_tensor(out=s1b4, in0=goh.unsqueeze(3).to_broadcast([P, NT, 4, 8]), in1=oh1.unsqueeze(2).to_broadcast([P, NT, 4, 8]),
                                      op=ALU.mult), R_, [B_rs])
        dve(lambda e: e.tensor_tensor(out=s2b4, in0=goh.unsqueeze(3).to_broadcast([P, NT, 4, 8]), in1=oh2.unsqueeze(2).to_broadcast([P, NT, 4, 8]),
                                      op=ALU.mult), R_, [B_rs])
        dve(lambda e: e.tensor_tensor(out=selb[:], in0=s1b[:], in1=s2b[:], op=ALU.add), [B_rs], [B_rs])

        for i in range(NT):
            fns = [lambda e, i=i: e.matmul(pb[3][:, i * 32:(i + 1) * 32], lhsT=Ustrict[:], rhs=selb3[:, i, :], start=True, stop=(i == 0))]
            for i2 in range(i):
                fns.append(lambda e, i=i, i2=i2: e.matmul(pb[3][:, i * 32:(i + 1) * 32], lhsT=ones[:], rhs=selb3[:, i2, :],
                                                          start=False, stop=(i2 == i - 1)))
            S.ops("pe", fns, reads=[B_rs, cB["Ustrict"], cB["ones"]], writes=[PB[3]])
        S.ops("pe", [lambda e, i=i: e.matmul(pb[2][:, 0:32], lhsT=ones[:], rhs=selb3[:, i, :], start=(i == 0), stop=(i == NT - 1))
                     for i in range(NT)], reads=[B_rs, cB["ones"]], writes=[PB[2]])
        MAGIC = 12582912.0
        pcA, pcB, pc, incl, off = rt2[:, 256:288], rt2[:, 288:320], rt2[:, 320:352], rt2[:, 352:384], rt2[:, 384:416]
        R_ = [B_rt]
        dve(lambda e: e.tensor_scalar(out=pcA, in0=pb[2][:, 0:32], scalar1=1.0 / 256, scalar2=(255.0 / 256 - 0.5 + 1.0 / 512),
                                      op0=ALU.mult, op1=ALU.add), [PB[2]], R_)
        dve(lambda e: e.tensor_scalar(out=pcB, in0=pcA, scalar1=MAGIC, scalar2=None, op0=ALU.add), R_, R_)
        dve(lambda e: e.tensor_scalar(out=pcA, in0=pcB, scalar1=-MAGIC, scalar2=None, op0=ALU.add), R_, R_)
        dve(lambda e: e.tensor_scalar(out=pc, in0=pcA, scalar1=256.0, scalar2=None, op0=ALU.mult), R_, R_)
        dve(lambda e: e.tensor_copy(out=pcA, in_=pc), R_, R_)
        src, dst = pcA, pcB
        for dd in (1, 2, 4, 8, 16):
            dve(lambda e, src=src, dst=dst, dd=dd: e.tensor_copy(out=dst[:, 0:dd], in_=src[:, 0:dd]), R_, R_)
            dve(lambda e, src=src, dst=dst, dd=dd: e.tensor_tensor(out=dst[:, dd:32], in0=src[:, dd:32], in1=src[:, 0:32 - dd], op=ALU.add), R_, R_)
            src, dst = dst, src
        dve(lambda e, src=src: e.tensor_copy(out=incl, in_=src), R_, R_)
        dve(lambda e: e.tensor_tensor(out=off, in0=incl, in1=pc, op=ALU.subtract), R_, R_)
        baseS = R4[:, 0:512]
        prodS = R4[:, 512:1024]
        cmpS = R4[:, 1024:2560]
        dve(lambda e: e.tensor_tensor(out=baseS.rearrange("p (i n) -> p i n", i=NT), in0=pb[3][:, :].rearrange("p (i n) -> p i n", i=NT),
                                      in1=off.unsqueeze(1).to_broadcast([P, NT, 32]), op=ALU.add), [PB[3]] + R_, [B_r4])
        for k, skb in ((0, s1b), (1, s2b)):
            dve(lambda e, skb=skb: e.tensor_tensor(out=prodS, in0=baseS, in1=skb[:], op=ALU.mult), [B_r4, B_rs], [B_r4])
            dve(lambda e, k=k: e.tensor_reduce(out=slotf[:, k * NT:(k + 1) * NT], in_=prodS.rearrange("p (i n) -> p i n", i=NT), axis=AX.X, op=ALU.add),
                [B_r4], [B_rs])
        dve(lambda e: e.tensor_copy(out=sloti[:], in_=slotf[:]), [B_rs], [B_rs])
        dve(lambda e: e.tensor_tensor(out=cmpS.rearrange("p (t n) -> p t n", t=NIT), in0=incl.unsqueeze(1).to_broadcast([P, NIT, 32]),
                                      in1=thr[:].unsqueeze(2).to_broadcast([P, NIT, 32]), op=ALU.is_le), R_ + [cB["thr"]], [B_r4])
        dve(lambda e: e.tensor_reduce(out=eitf[:], in_=cmpS.rearrange("p (t n) -> p t n", t=NIT), axis=AX.X, op=ALU.add), [B_r4], [B_rs])
        dve(lambda e: e.tensor_scalar(out=eitf[:], in0=eitf[:], scalar1=256.0, scalar2=p2[:, 0:1], op0=ALU.mult, op1=ALU.add),
            [B_rs, cB["p2"]], [B_rs])
        widx3 = widx[:].rearrange("p (t a) -> p t a", a=2)
        dve(lambda e: e.tensor_copy(out=widx3[:, :, 0], in_=eitf[:]), [B_rs], [B_rs])
        dve(lambda e: e.tensor_scalar(out=eitf[:], in0=eitf[:], scalar1=1.0, scalar2=None, op0=ALU.add), [B_rs], [B_rs])
        dve(lambda e: e.tensor_copy(out=widx3[:, :, 1], in_=eitf[:]), [B_rs], [B_rs])

        scat_toks = []
        for i in range(NT):
            for k in range(2):
                scat_toks.append(S.idma(out=xs[:, :], out_off=sloti[:, k * NT + i:k * NT + i + 1], in_=hn2b[:, i, :], in_off=None,
                                        sbuf_buf=B_hn2b[i], reads=[B_hn2b[i], B_rs], writes=[], bounds_check=NSLOT - 1))

        if stage == "sp_dbg":
            dbg = R2[:, 8192:9216]
            B_dbg = Buf("dbg", after=B_hn2f2 + B_hn2Tf2)
            dve(lambda e: e.memset(dbg, 0.0), [], [B_dbg])
            for (c0, n, src, rd) in ((0, 32, slotf[:], [B_rs]), (32, 32, sloti[:], [B_rs]), (64, 48, eitf[:], [B_rs]), (112, 96, widx[:], [B_rs]),
                                     (208, 32, pc, [B_rt]), (240, 32, incl, [B_rt]), (272, 32, off, [B_rt]), (304, 32, pb[2][:, 0:32], [PB[2]]),
                                     (336, 32, wts[:], [B_rs]), (368, 512, pb[3][:, :], [PB[3]])):
                dve(lambda e, c0=c0, n=n, src=src: e.tensor_copy(out=dbg[:, c0:c0 + n], in_=src), rd, [B_dbg])
            t = S.dma("sp", y_t[0], dbg, B_dbg, reads=[B_dbg])
            S.wait_tok("sp", t)
            for t in scat_toks:
                S.wait_tok("sp", t)
            return nc
        if stage == "sp_scatter":
            for t in scat_toks:
                S.wait_tok("sp", t)
            finish(lambda i: h1[:, i, :], B_h1)
            return nc
        old_R3 = allq + [b for row in B_k for b in row] + B_v + B_msb
        wg_sb, wu_sb, wd_sb, B_wg, B_wu, B_wd, wflat = [], [], [], [], [], [], []
        NSL = 2
        for sl in range(NSL):
            o = sl * 6144
            RR = R3 if sl < 2 else R2
            o = o if sl < 2 else 0
            fl = [bf(RR[:, o + m * 2048:o + (m + 1) * 2048]) for m in range(3)]
            wflat.append(fl)
            wg_sb.append(fl[0].rearrange("p (c n) -> p c n", c=8))
            wu_sb.append(fl[1].rearrange("p (c n) -> p c n", c=8))
            wd_sb.append(fl[2].rearrange("p (c n) -> p c n", c=4))
            aft = old_R3 if sl < 2 else B_hn2b
            B_wg.append(Buf("wg%d" % sl, after=aft))
            B_wu.append(Buf("wu%d" % sl, after=aft))
            B_wd.append(Buf("wd%d" % sl, after=aft))
        xin = [bf(R4[:, k * 1024:(k + 1) * 1024]).rearrange("p (a d) -> p a d", a=2) for k in range(2)]
        B_xin = [Buf("xin%d" % k, after=[B_r4]) for k in range(2)]
        xT = bf(R4[:, 2048:3072]).rearrange("p (c n) -> p c n", c=8)
        B_xT = Buf("xT", after=[B_r4])
        actT = bf(R4[:, 3072:3584]).rearrange("p (c n) -> p c n", c=4)
        B_actT = [Buf("actT%d" % c, after=[B_r4]) for c in range(4)]
        sgT = [R4[:, 3584 + k * 256:3840 + k * 256] for k in range(2)]
        B_sgT = [Buf("sgT%d" % k, after=[B_r4]) for k in range(2)]
        ysb = [R2[:, 8192 + k * 2048:10240 + k * 2048].rearrange("p (a d) -> p a d", a=2) for k in range(2)]
        B_ysb = [Buf("ysb%d" % k, after=B_hn2f2 + B_hn2Tf2) for k in range(2)]
        PBh = [[Buf("pb%d_h%d" % (b, h), after=[PB[b]]) for h in range(2)] for b in (2, 3)]
        xs_t = xs.rearrange("(t a p) d -> t p a d", a=2, p=P)
        ys_t = ys.rearrange("(t a p) d -> t p a d", a=2, p=P)

        NSTG = 4
        stg = [R2[:, k * 2048:(k + 1) * 2048] for k in range(NSTG)]
        B_stg = [Buf("stg%d" % k, after=B_hn2b) for k in range(NSTG)]
        pieces = [(it_, m_, a_) for it_ in range(NRUN) for m_ in range(3) for a_ in range(2)]
        wdrams = (w_gate, w_up, w_down)

        def gather_piece(g):
            if g >= len(pieces):
                return
            it_, m_, a_ = pieces[g]
            sg_ = g % NSTG
            S.idma(out=stg[sg_], out_off=None, in_=wdrams[m_][:, :], in_off=widx[:, it_ * 2 + a_:it_ * 2 + a_ + 1],
                   sbuf_buf=B_stg[sg_], reads=[B_rs], writes=[B_stg[sg_]], bounds_check=8191)

        def cast_piece(it, j):
            if it >= NRUN:
                return
            g = it * 6 + j
            it_, m, a = pieces[g]
            sg_ = g % NSTG
            sl = it % NSL
            Bw = (B_wg, B_wu, B_wd)[m][sl]
            dst = wflat[sl][m][:, a * 2048:(a + 1) * 2048]
            if j % 2 == 0:
                S.op("act", lambda e, dst=dst, sg_=sg_: e.activation(out=dst, in_=stg[sg_], func=AF.Copy), reads=[B_stg[sg_]], writes=[Bw])
            else:
                S.op("dve", lambda e, dst=dst, sg_=sg_: e.tensor_copy(out=dst, in_=stg[sg_]), reads=[B_stg[sg_]], writes=[Bw])
            gather_piece(g + NSTG)

        def load_w(it):
            if "n" in DBG:
                RR = R3 if it % 2 == 0 else R2
                Bx = B_nocast[it % 2]
                for m, wdram in enumerate((w_gate, w_up, w_down)):
                    for a in range(2):
                        o = (m * 2 + a) * 2048
                        S.idma(out=RR[:, o:o + 2048], out_off=None, in_=wdram[:, :], in_off=widx[:, it * 2 + a:it * 2 + a + 1],
                               sbuf_buf=Bx[m * 2 + a], reads=[B_rs], writes=[Bx[m * 2 + a]], bounds_check=8191)
                return
            sl = it % NSL
            for m, (wdram, Bw) in enumerate(((w_gate, B_wg[sl]), (w_up, B_wu[sl]), (w_down, B_wd[sl]))):
                for a in range(2):
                    S.idma(out=wflat[sl][m][:, a * 2048:(a + 1) * 2048], out_off=None, in_=wdram[:, :], in_off=widx[:, it * 2 + a:it * 2 + a + 1],
                           sbuf_buf=Bw, reads=[B_rs], writes=[Bw], bounds_check=8191)

        def load_x(it):
            k = it % 2
            S.dma("sp", xin[k], xs_t[it], B_xin[k], writes=[B_xin[k]])

        def TR(it):
            k = it % 2
            for a in range(2):
                tpa = bf(pb[a][:])
                S.ops("pe", [lambda e, c=c, a=a, k=k, tpa=tpa: e.transpose(out=tpa[:, c * 128:(c + 1) * 128], in_=xin[k][:, a, c * 128:(c + 1) * 128],
                                                                          identity=ident[:]) for c in range(8)],
                      reads=[B_xin[k], cB["ident"]], writes=[PB[a]])

        def TR_evac(it):
            for a in range(2):
                tpa = bf(pb[a][:]).rearrange("p (c n) -> p c n", c=8)
                if a == 0:
                    S.op("act", lambda e, tpa=tpa, a=a: e.activation(out=xT[:, :, a * 128:(a + 1) * 128], in_=tpa, func=AF.Copy),
                         reads=[PB[a]], writes=[B_xT])
                else:
                    S.op("dve", lambda e, tpa=tpa, a=a: e.tensor_copy(out=xT[:, :, a * 128:(a + 1) * 128], in_=tpa),
                         reads=[PB[a]], writes=[B_xT])

        def GU(it):
            sl = it % NSL
            for fc in range(4):
                h = fc % 2
                bk = 2 + h
                S.ops("pe", [lambda e, c=c, sl=sl, fc=fc, bk=bk: e.matmul(pb[bk][:, 0:256], lhsT=wg_sb[sl][:, c, fc * 128:(fc + 1) * 128],
                                                                         rhs=xT[:, c, :], start=(c == 0), stop=(c == 7)) for c in range(8)] +
                            [lambda e, c=c, sl=sl, fc=fc, bk=bk: e.matmul(pb[bk][:, 256:512], lhsT=wu_sb[sl][:, c, fc * 128:(fc + 1) * 128],
                                                                         rhs=xT[:, c, :], start=(c == 0), stop=(c == 7)) for c in range(8)],
                      reads=[B_wg[sl], B_wu[sl], B_xT], writes=[PB[bk]])
                S.op("act", lambda e, h=h, bk=bk: e.activation(out=sgT[h], in_=pb[bk][:, 0:256], func=AF.Silu),
                     reads=[PB[bk]], writes=[B_sgT[h]])
                S.op("dve", lambda e, h=h, fc=fc, bk=bk: e.tensor_tensor(out=actT[:, fc, :], in0=pb[bk][:, 256:512], in1=sgT[h], op=ALU.mult),
                     reads=[PB[bk], B_sgT[h]], writes=[B_actT[fc]])
                cast_piece(it + 1, fc)

        y_toks = []
        B_nocast = [[Buf("nc%d_%d" % (k, m), after=old_R3 + B_hn2b + B_hn2f2 + B_hn2Tf2) for m in range(6)] for k in range(2)]

        def DOWN(it):
            sl, k = it % NSL, it % 2
            for a in range(2):
                for half in range(2):
                    by = 4 + a * 2 + half
                    S.ops("pe", [lambda e, fc=fc, sl=sl, a=a, half=half, by=by: e.matmul(
                        pb[by][:, :], lhsT=actT[:, fc, a * 128:(a + 1) * 128], rhs=wd_sb[sl][:, fc, half * 512:(half + 1) * 512],
                        start=(fc == 0), stop=(fc == 3)) for fc in range(4)], reads=B_actT + [B_wd[sl]], writes=[PB[by]])
                    if half == 0:
                        S.op("act", lambda e, a=a, half=half, by=by, k=k: e.activation(out=ysb[k][:, a, half * 512:(half + 1) * 512], in_=pb[by][:, :],
                                                                                     func=AF.Copy), reads=[PB[by]], writes=[B_ysb[k]])
                    else:
                        S.op("dve", lambda e, a=a, half=half, by=by, k=k: e.tensor_copy(out=ysb[k][:, a, half * 512:(half + 1) * 512], in_=pb[by][:, :]),
                             reads=[PB[by]], writes=[B_ysb[k]])
                cast_piece(it + 1, 4 + a)
            if "y" in DBG:
                y_toks.append(S.dma("sp", ys_t[it], ysb[k], B_ysb[k], reads=[B_ysb[k]]))

        for g in range(NSTG):
            gather_piece(g)
        for j in range(6):
            cast_piece(0, j)
        for t in scat_toks:
            S.wait_tok("sp", t)
        load_x(0)
        load_x(1)
        TR(0)
        TR_evac(0)
        for it in range(NRUN):
            GU(it)
            if it + 1 < NRUN:
                TR(it + 1)
            DOWN(it)
            if it + 1 < NRUN:
                TR_evac(it + 1)
            if it + 2 < NRUN:
                load_x(it + 2)
        if stage == "sp_moe":
            for t in y_toks:
                S.wait_tok("sp", t)
            finish(lambda i: h1[:, i, :], B_h1)
            return nc
        NYG = 6
        yg = [[R3[:, (k * 2 + j) * 1024:(k * 2 + j + 1) * 1024] for j in range(2)] for k in range(NYG)]
        B_yg = [[Buf("yg%d_%d" % (k, j), after=B_wg + B_wu + B_wd) for j in range(2)] for k in range(NYG)]
        oring = [R2[:, k * 1024:(k + 1) * 1024] for k in range(2)]
        B_or = [Buf("or%d" % k, after=B_hn2b + B_stg) for k in range(2)]
        for t in y_toks:
            S.wait_tok("pool", t)
        out_toks = []
        for i in range(NT):
            k = i % NYG
            for j in range(2):
                S.idma(out=yg[k][j], out_off=None, in_=ys[:, :], in_off=sloti[:, j * NT + i:j * NT + i + 1], sbuf_buf=B_yg[k][j],
                       reads=[B_rs], writes=[B_yg[k][j]], bounds_check=NSLOT - 1)
            for j in range(2):
                S.op("dve", lambda e, i=i, j=j, k=k: e.scalar_tensor_tensor(out=h1[:, i, :], in0=yg[k][j], scalar=wts3[:, i, j:j + 1], in1=h1[:, i, :],
                                                                            op0=ALU.mult, op1=ALU.add),
                     reads=[B_yg[k][j], B_rs, B_h1[i]], writes=[B_h1[i]])
            ko = i % 2
            c8 = 16 + 2 * i
            S.op("act", lambda e, i=i, ko=ko, c8=c8: e.activation(out=oring[ko], in_=h1[:, i, :], func=AF.Square, accum_out=stat[:, c8:c8 + 1]),
                 reads=[B_h1[i]], writes=[B_or[ko], B_st6[i]])
            rstd_act(c8, D, B_st6[i])
            S.op("dve", lambda e, i=i, ko=ko, c8=c8: e.scalar_tensor_tensor(out=oring[ko], in0=h1[:, i, :], scalar=stat[:, c8 + 1:c8 + 2], in1=gb_fin[:],
                                                                            op0=ALU.mult, op1=ALU.mult),
                 reads=[B_h1[i], B_st6[i], cB["gb_fin"]], writes=[B_or[ko]])
            out_toks.append(S.dma("sp", y_t[i], oring[ko], B_or[ko], reads=[B_or[ko]]))
        for t in out_toks:
            S.wait_tok("sp", t)
    return nc


def _prep_inputs(inputs):
    f = lambda a: np.ascontiguousarray(np.asarray(a, dtype=np.float32))
    w_r = np.concatenate([f(inputs["w_router_group"])[0],
                          np.transpose(f(inputs["w_router_expert"])[0], (1, 0, 2)).reshape(D, 32)], axis=1)
    b_r = np.concatenate([f(inputs["b_router_group"])[0], f(inputs["b_router_expert"])[0].reshape(32)], axis=0)
    shared = {
        "w_in": f(inputs["w_in"])[0],
        "w_out": f(inputs["w_out"])[0],
        "w_gate": np.ascontiguousarray(f(inputs["w_gate"])[0].reshape(32, 8, 128, 512).transpose(0, 2, 1, 3)).reshape(8192, 2048),
        "w_up": np.ascontiguousarray(f(inputs["w_up"])[0].reshape(32, 8, 128, 512).transpose(0, 2, 1, 3)).reshape(8192, 2048),
        "w_down": np.ascontiguousarray(f(inputs["w_down"])[0].reshape(32, 4, 128, 1024).transpose(0, 2, 1, 3)).reshape(8192, 2048),
        "w_spatial": f(inputs["w_spatial"])[0],
        "b_spatial": f(inputs["b_spatial"])[0],
        "attn_norm_g": f(inputs["attn_norm_g"])[0],
        "ffn_norm_g": f(inputs["ffn_norm_g"])[0],
        "final_norm_g": f(inputs["final_norm_g"]),
        "sg_norm_g": f(inputs["sg_norm_g"])[0],
        "sb_out_norm_g": f(inputs["sb_out_norm_g"])[0],
        "sg_out_norm_g": f(inputs["sg_out_norm_g"])[0],
        "w_router": np.ascontiguousarray(w_r),
        "b_router": np.ascontiguousarray(b_r),
    }
    xs = f(inputs["x"])
    return [dict(shared, x=np.ascontiguousarray(xs[b])) for b in range(8)]


def kernel(_stage="full", **inputs):
    in_maps = _prep_inputs(inputs)
    nc = build_nc(_stage)
    res = run_bass_kernel_spmd(nc, in_maps, core_ids=list(range(8)))
    return np.stack([np.asarray(r["y"], dtype=np.float32) for r in res.results], axis=0)
```
